# Optimizing a Trainium2 kernel written in Bass

```python
import math
import jax
import jax.numpy as jnp
from jax import lax
import numpy as np

D_MODEL = 1024
BATCH = 4
SEQ = 4096
DEPTH = 4

GRID_W = 64
CTX_LEN = 256
N_EVEN = (DEPTH + 1) // 2
N_ODD = DEPTH // 2
NORM_EPS = 1e-6
N_MOD = 6

RW_WIDTH = D_MODEL // 2
RW_HEAD = 64
RW_HEADS = RW_WIDTH // RW_HEAD
RW_DECAY_RANK = 32
RW_A_RANK = 32
RW_GATE_RANK = 96
RW_GN_EPS = 64e-5
RW_SPLITS = (RW_WIDTH, 2 * RW_WIDTH, 3 * RW_WIDTH, 3 * RW_WIDTH + 2 * RW_DECAY_RANK, 3 * RW_WIDTH + 2 * RW_DECAY_RANK + 2 * RW_A_RANK)
RW_COLS = 3 * RW_WIDTH + 2 * RW_DECAY_RANK + 2 * RW_A_RANK + RW_GATE_RANK

HY_WIDTH = D_MODEL - RW_WIDTH
HY_ORDER = 2
HY_EMB = 33
HY_BANDS = (HY_EMB - 1) // 2
HY_FILTER_HIDDEN = 64
HY_DECAY_TARGET = 1e-2
HY_FAST_DECAY = 0.3
HY_SLOW_DECAY = 1.5
HY_COLS = (HY_ORDER + 1) * HY_WIDTH
EVEN_COLS = RW_COLS + HY_COLS

DA_HEADS = 8
DA_HEAD = 64
DA_QCOLS = DA_HEADS * 2 * DA_HEAD
DA_COLS = 3 * DA_QCOLS
ROPE_BASE = 10000.0
Q_BLOCK = 128

N_EXPERTS = 16
EXPERT_HIDDEN = 1024
CAPACITY_FACTOR = 2

kernel_name = "hybrid_rwkv7_hyena_diffattn_ecmoe_dit"


def rms_norm(x, g, eps=NORM_EPS):
    x32 = x.astype(jnp.float32)
    y = x32 * lax.rsqrt(jnp.mean(x32 * x32, axis=-1, keepdims=True) + eps)
    return (y * g.astype(jnp.float32)).astype(x.dtype)


def modulate(x, g, shift, scale):
    return rms_norm(x, g) * (1 + scale[..., None, :]) + shift[..., None, :]


def rwkv_inputs(u, mu, w0, w2, a0, a2, g2, k_k, k_a):
    B, L, _ = u.shape
    u_prev = jnp.pad(u, ((0, 0), (1, 0), (0, 0)))[:, :-1]
    u_next = jnp.pad(u, ((0, 0), (0, 1), (0, 0)))[:, 1:]
    u = u + mu[0] * (u_prev - u) + mu[1] * (u_next - u)
    r, k, v, wl, al, gl = jnp.split(u, list(RW_SPLITS), axis=-1)
    wl = wl.reshape(B, L, 2, RW_DECAY_RANK)
    al = al.reshape(B, L, 2, RW_A_RANK)
    w = w0 + jnp.einsum('bldr,drc->bldc', jnp.tanh(wl), w2)
    decay = jnp.exp(-jnp.exp((-jax.nn.softplus(-w) - 0.5).astype(jnp.float32)))
    a = jax.nn.sigmoid(a0 + jnp.einsum('bldr,drc->bldc', al, a2))
    g = jax.nn.sigmoid(gl) @ g2
    kk = (k * k_k).reshape(B, L, RW_HEADS, RW_HEAD).astype(jnp.float32)
    kk = kk / jnp.maximum(jnp.sqrt(jnp.sum(kk * kk, axis=-1, keepdims=True)), 1e-12)
    k_dir = k[:, :, None] * (1 + (a - 1) * k_a)
    hd = lambda t: t.reshape(*t.shape[:-1], RW_HEADS, RW_HEAD)
    return hd(r), hd(decay), hd(k_dir), hd(v), kk, hd(a), g


def wkv7_scan(r, decay, k, v, kk, a, s0, reverse):
    def step(s, inp):
        r_t, w_t, k_t, v_t, kk_t, a_t = inp
        sa = jnp.einsum('bhij,bhj->bhi', s, kk_t)
        s = (s * w_t[:, :, None, :]
             - sa[..., None] * (kk_t * a_t)[:, :, None, :]
             + v_t[..., None] * k_t[:, :, None, :])
        return s, jnp.einsum('bhij,bhj->bhi', s, r_t)
    xs = tuple(jnp.moveaxis(t.astype(jnp.float32), 1, 0) for t in (r, decay, k, v, kk, a))
    s_fin, ys = lax.scan(step, s0, xs, reverse=reverse)
    return jnp.moveaxis(ys, 0, 1), s_fin


def rwkv_readout(y, r, k_dir, v, g, r_k, gn):
    B, L = y.shape[:2]
    mu = jnp.mean(y, axis=-1, keepdims=True)
    var = jnp.mean(jnp.square(y - mu), axis=-1, keepdims=True)
    yn = ((y - mu) * lax.rsqrt(var + RW_GN_EPS)).reshape(B, L, RW_WIDTH) * gn[0] + gn[1]
    bonus = jnp.sum(r[:, :, None] * k_dir * r_k, axis=(2, 4))
    out = yn + (bonus[..., None] * v).reshape(B, L, RW_WIDTH)
    return (out * g).astype(g.dtype)


def hyena_filters(L, w1, b1, w2, b2, w3, freq):
    f32 = jnp.float32
    t = jnp.linspace(0.0, 1.0, L, dtype=f32)[:, None]
    pos = jnp.arange(L, dtype=f32)[:, None]
    bands = jnp.linspace(1e-4, HY_BANDS - 1, HY_BANDS, dtype=f32)[None, :]
    ang = 2.0 * math.pi * pos * bands / L
    z = jnp.concatenate([t, jnp.cos(ang), -jnp.sin(ang)], axis=-1)
    hdn = jnp.sin(freq[0].astype(f32) * (z @ w1.astype(f32) + b1.astype(f32)))
    hdn = jnp.sin(freq[1].astype(f32) * (hdn @ w2.astype(f32) + b2.astype(f32)))
    h = (hdn @ w3.astype(f32)).reshape(L, HY_ORDER, 2, HY_WIDTH)
    deltas = jnp.linspace(math.log(HY_DECAY_TARGET) / HY_SLOW_DECAY,
                          math.log(HY_DECAY_TARGET) / HY_FAST_DECAY, HY_WIDTH, dtype=f32)
    h = h * jnp.exp(-t[:, :, None, None] * jnp.abs(deltas))
    fwd = h[:, :, 0]
    bwd = h[1:, :, 1][::-1]
    norm = jnp.sum(jnp.abs(fwd), axis=0) + jnp.sum(jnp.abs(bwd), axis=0) + 1e-6
    kc = jnp.concatenate([fwd, jnp.zeros((1, HY_ORDER, HY_WIDTH), f32), bwd], axis=0) / norm
    return jnp.fft.rfft(kc, axis=0)


def fft_long_conv(u, kf):
    L = u.shape[1]
    uf = jnp.fft.rfft(u.astype(jnp.float32), n=2 * L, axis=1)
    y = jnp.fft.irfft(uf * kf[None], n=2 * L, axis=1)[:, :L]
    return y.astype(u.dtype)


def hyena_mixer(u, conv_w, conv_b, f_w1, f_b1, f_w2, f_b2, f_w3, f_freq, hy_b):
    L = u.shape[1]
    up = jnp.pad(u, ((0, 0), (1, 1), (0, 0)))
    u = up[:, :-2] * conv_w[0] + up[:, 1:-1] * conv_w[1] + up[:, 2:] * conv_w[2] + conv_b
    v, x1, x2 = jnp.split(u, HY_ORDER + 1, axis=-1)
    kf = hyena_filters(L, f_w1, f_b1, f_w2, f_b2, f_w3, f_freq)
    z = x1 * (fft_long_conv(v, kf[:, 0]) + v * hy_b[0])
    return x2 * (fft_long_conv(z, kf[:, 1]) + z * hy_b[1])


def even_mixer(h_lat, h_ctx, w_in, w_out, mu, w0, w2, a0, a2, g2, k_k, k_a, r_k, gn,
               conv_w, conv_b, f_w1, f_b1, f_w2, f_b2, f_w3, f_freq, hy_b, need_ctx):
    p_lat = h_lat @ w_in
    p_ctx = h_ctx @ w_in
    lat = rwkv_inputs(p_lat[..., :RW_COLS], mu, w0, w2, a0, a2, g2, k_k, k_a)
    ctx = rwkv_inputs(p_ctx[..., :RW_COLS], mu, w0, w2, a0, a2, g2, k_k, k_a)
    s0 = jnp.zeros((h_lat.shape[0], RW_HEADS, RW_HEAD, RW_HEAD), jnp.float32)
    ys_lat, ys_ctx = [], []
    for d in range(2):
        rev = d == 1
        yc, sc = wkv7_scan(ctx[0], ctx[1][:, :, d], ctx[2][:, :, d], ctx[3], ctx[4], ctx[5][:, :, d], s0, rev)
        yl, _ = wkv7_scan(lat[0], lat[1][:, :, d], lat[2][:, :, d], lat[3], lat[4], lat[5][:, :, d], sc, rev)
        ys_lat.append(yl)
        ys_ctx.append(yc)
    rw_lat = rwkv_readout(ys_lat[0] + ys_lat[1], lat[0], lat[2], lat[3], lat[6], r_k, gn)
    hy_lat = hyena_mixer(p_lat[..., RW_COLS:], conv_w, conv_b, f_w1, f_b1, f_w2, f_b2, f_w3, f_freq, hy_b)
    o_lat = jnp.concatenate([rw_lat, hy_lat], axis=-1) @ w_out
    o_ctx = None
    if need_ctx:
        rw_ctx = rwkv_readout(ys_ctx[0] + ys_ctx[1], ctx[0], ctx[2], ctx[3], ctx[6], r_k, gn)
        hy_ctx = hyena_mixer(p_ctx[..., RW_COLS:], conv_w, conv_b, f_w1, f_b1, f_w2, f_b2, f_w3, f_freq, hy_b)
        o_ctx = jnp.concatenate([rw_ctx, hy_ctx], axis=-1) @ w_out
    return o_lat, o_ctx


def axial_rope(L):
    rows = L // GRID_W
    row = jnp.repeat(jnp.arange(rows, dtype=jnp.float32), GRID_W)
    col = jnp.tile(jnp.arange(GRID_W, dtype=jnp.float32), rows)
    n_freq = DA_HEAD // 4
    inv = ROPE_BASE ** (-jnp.arange(n_freq, dtype=jnp.float32) / n_freq)
    ang = jnp.stack([row[:, None] * inv, col[:, None] * inv], axis=1)
    return jnp.cos(ang), jnp.sin(ang)


def apply_rope(x, cos, sin):
    shp = x.shape
    xr = x.reshape(*shp[:-1], 2, 2, DA_HEAD // 4).astype(jnp.float32)
    x1, x2 = xr[..., 0, :], xr[..., 1, :]
    c = cos[None, :, None, None]
    s = sin[None, :, None, None]
    out = jnp.stack([x1 * c - x2 * s, x2 * c + x1 * s], axis=-2)
    return out.reshape(shp).astype(x.dtype)


def diff_attend(q, k, v, lam):
    s = jnp.einsum('bqhcd,bkhcd->bhcqk', q, k).astype(jnp.float32) * (DA_HEAD ** -0.5)
    p = jax.nn.softmax(s, axis=-1)
    w = p[:, :, 0] - lam * p[:, :, 1]
    return jnp.einsum('bhqk,bkhe->bqhe', w.astype(v.dtype), v)


def diff_attn_mixer(h_lat, h_ctx, w_qkv, w_out, lam_vecs, subln_g, lambda_init, need_ctx):
    B, L, _ = h_lat.shape
    Lc = h_ctx.shape[1]
    p = h_lat @ w_qkv
    q = p[..., :DA_QCOLS].reshape(B, L, DA_HEADS, 2, DA_HEAD)
    k = p[..., DA_QCOLS:2 * DA_QCOLS].reshape(B, L, DA_HEADS, 2, DA_HEAD)
    v = p[..., 2 * DA_QCOLS:].reshape(B, L, DA_HEADS, 2 * DA_HEAD)
    cos, sin = axial_rope(L)
    q = apply_rope(q, cos, sin)
    k = apply_rope(k, cos, sin)
    kv_c = h_ctx @ w_qkv[:, DA_QCOLS:]
    k_c = kv_c[..., :DA_QCOLS].reshape(B, Lc, DA_HEADS, 2, DA_HEAD)
    v_c = kv_c[..., DA_QCOLS:].reshape(B, Lc, DA_HEADS, 2 * DA_HEAD)
    lv = lam_vecs.astype(jnp.float32)
    lam = jnp.exp(jnp.sum(lv[0] * lv[1])) - jnp.exp(jnp.sum(lv[2] * lv[3])) + lambda_init
    keys = jnp.concatenate([k_c, k], axis=1)
    vals = jnp.concatenate([v_c, v], axis=1)
    nb = L // Q_BLOCK
    qb = jnp.moveaxis(q.reshape(B, nb, Q_BLOCK, DA_HEADS, 2, DA_HEAD), 1, 0)
    o = lax.map(lambda qq: diff_attend(qq, keys, vals, lam), qb)
    o = jnp.moveaxis(o, 0, 1).reshape(B, L, DA_HEADS, 2 * DA_HEAD)

    def finish(oo):
        oo = rms_norm(oo, subln_g, 1e-5) * (1 - lambda_init)
        return oo.reshape(*oo.shape[:2], DA_HEADS * 2 * DA_HEAD) @ w_out

    o_lat = finish(o)
    o_ctx = None
    if need_ctx:
        q_c = (h_ctx @ w_qkv[:, :DA_QCOLS]).reshape(B, Lc, DA_HEADS, 2, DA_HEAD)
        o_ctx = finish(diff_attend(q_c, k_c, v_c, lam))
    return o_lat, o_ctx


def expert_choice_ffn(h, router_w, w1, w3, w2):
    B, L, D = h.shape
    cap = CAPACITY_FACTOR * L // N_EXPERTS
    aff = jax.nn.softmax((h @ router_w).astype(jnp.float32), axis=-1)
    gate, idx = lax.top_k(jnp.swapaxes(aff, 1, 2), cap)
    xg = jax.vmap(lambda hb, ib: hb[ib])(h, idx)
    a = jnp.einsum('becd,edf->becf', xg, w1)
    b = jnp.einsum('becd,edf->becf', xg, w3)
    y = jnp.einsum('becf,efd->becd', jax.nn.silu(a) * b, w2) * gate[..., None].astype(h.dtype)
    return jax.vmap(lambda yb, ib: jax.ops.segment_sum(yb.reshape(-1, D), ib.reshape(-1), num_segments=L))(y, idx)


def setup_inputs(seed: int = 0) -> dict:
    key = jax.random.key(seed)
    keys = iter(jax.random.split(key, 40))
    f32 = jnp.float32

    def nrm(shape, scale):
        return jax.random.normal(next(keys), shape, f32) * scale

    def unif(shape, lo, hi):
        return jax.random.uniform(next(keys), shape, f32, lo, hi)

    D, E, F = D_MODEL, N_EXPERTS, EXPERT_HIDDEN
    return {
        "x": nrm((BATCH, SEQ, D), 1.0),
        "c": nrm((BATCH, D), 1.0),
        "ctx": nrm((BATCH, CTX_LEN, D), 1.0),
        "c_ctx": nrm((D,), 1.0),
        "ada_w": nrm((DEPTH, D, N_MOD * D), 0.5 * D ** -0.5),
        "ada_b": nrm((DEPTH, N_MOD * D), 0.02),
        "norm_g": 1.0 + nrm((DEPTH, 2, D), 0.02),
        "final_g": 1.0 + nrm((D,), 0.02),
        "ev_w_in": nrm((N_EVEN, D, EVEN_COLS), D ** -0.5),
        "ev_w_out": nrm((N_EVEN, RW_WIDTH + HY_WIDTH, D), (RW_WIDTH + HY_WIDTH) ** -0.5),
        "rw_mu": unif((N_EVEN, 2, RW_COLS), 0.0, 0.5),
        "rw_w0": unif((N_EVEN, 2, RW_WIDTH), -6.0, 0.0),
        "rw_w2": nrm((N_EVEN, 2, RW_DECAY_RANK, RW_WIDTH), 0.1 * RW_DECAY_RANK ** -0.5),
        "rw_a0": nrm((N_EVEN, 2, RW_WIDTH), 0.5),
        "rw_a2": nrm((N_EVEN, 2, RW_A_RANK, RW_WIDTH), 0.3 * RW_A_RANK ** -0.5),
        "rw_g2": nrm((N_EVEN, RW_GATE_RANK, RW_WIDTH), RW_GATE_RANK ** -0.5),
        "rw_kk": 0.85 + nrm((N_EVEN, RW_WIDTH), 0.05),
        "rw_ka": 1.0 + nrm((N_EVEN, RW_WIDTH), 0.05),
        "rw_rk": nrm((N_EVEN, RW_HEADS, RW_HEAD), 0.1),
        "rw_gn": nrm((N_EVEN, 2, RW_WIDTH), 0.02) + jnp.array([1.0, 0.0], f32)[:, None],
        "hy_conv_w": nrm((N_EVEN, 3, HY_COLS), 3 ** -0.5),
        "hy_conv_b": nrm((N_EVEN, HY_COLS), 0.02),
        "hy_f_w1": nrm((N_EVEN, HY_EMB, HY_FILTER_HIDDEN), HY_EMB ** -0.5),
        "hy_f_b1": nrm((N_EVEN, HY_FILTER_HIDDEN), 0.5),
        "hy_f_w2": nrm((N_EVEN, HY_FILTER_HIDDEN, HY_FILTER_HIDDEN), HY_FILTER_HIDDEN ** -0.5),
        "hy_f_b2": nrm((N_EVEN, HY_FILTER_HIDDEN), 0.5),
        "hy_f_w3": nrm((N_EVEN, HY_FILTER_HIDDEN, HY_ORDER * 2 * HY_WIDTH), HY_FILTER_HIDDEN ** -0.5),
        "hy_freq": 1.0 + nrm((N_EVEN, 2, HY_FILTER_HIDDEN), 0.1),
        "hy_bias": nrm((N_EVEN, HY_ORDER, HY_WIDTH), 0.5),
        "da_w_qkv": nrm((N_ODD, D, DA_COLS), D ** -0.5),
        "da_w_out": nrm((N_ODD, DA_QCOLS, D), DA_QCOLS ** -0.5),
        "da_lambda": nrm((N_ODD, 4, DA_HEAD), 0.1),
        "da_subln": 1.0 + nrm((N_ODD, 2 * DA_HEAD), 0.02),
        "moe_router": nrm((DEPTH, D, E), D ** -0.5),
        "moe_w1": nrm((DEPTH, E, D, F), D ** -0.5),
        "moe_w3": nrm((DEPTH, E, D, F), D ** -0.5),
        "moe_w2": nrm((DEPTH, E, F, D), F ** -0.5),
    }


def reference(x, c, ctx, c_ctx, ada_w, ada_b, norm_g, final_g, ev_w_in, ev_w_out,
              rw_mu, rw_w0, rw_w2, rw_a0, rw_a2, rw_g2, rw_kk, rw_ka, rw_rk, rw_gn,
              hy_conv_w, hy_conv_b, hy_f_w1, hy_f_b1, hy_f_w2, hy_f_b2, hy_f_w3, hy_freq, hy_bias,
              da_w_qkv, da_w_out, da_lambda, da_subln, moe_router, moe_w1, moe_w3, moe_w2):
    x_lat, x_ctx = x, ctx
    s_lat = jax.nn.silu(c)
    s_ctx = jax.nn.silu(c_ctx)
    for i in range(DEPTH):
        j = i // 2
        need_ctx = i < DEPTH - 1
        m_lat = jnp.split(s_lat @ ada_w[i] + ada_b[i], N_MOD, axis=-1)
        m_ctx = jnp.split(s_ctx @ ada_w[i] + ada_b[i], N_MOD, axis=-1)
        h_lat = modulate(x_lat, norm_g[i, 0], m_lat[0], m_lat[1])
        h_ctx = modulate(x_ctx, norm_g[i, 0], m_ctx[0], m_ctx[1])
        if i % 2 == 0:
            o_lat, o_ctx = even_mixer(h_lat, h_ctx, ev_w_in[j], ev_w_out[j], rw_mu[j], rw_w0[j], rw_w2[j],
                                      rw_a0[j], rw_a2[j], rw_g2[j], rw_kk[j], rw_ka[j], rw_rk[j], rw_gn[j],
                                      hy_conv_w[j], hy_conv_b[j], hy_f_w1[j], hy_f_b1[j], hy_f_w2[j],
                                      hy_f_b2[j], hy_f_w3[j], hy_freq[j], hy_bias[j], need_ctx)
        else:
            lambda_init = 0.8 - 0.6 * math.exp(-0.3 * i)
            o_lat, o_ctx = diff_attn_mixer(h_lat, h_ctx, da_w_qkv[j], da_w_out[j], da_lambda[j],
                                           da_subln[j], lambda_init, need_ctx)
        x_lat = x_lat + m_lat[2][:, None, :] * o_lat
        x_lat = x_lat + m_lat[5][:, None, :] * expert_choice_ffn(
            modulate(x_lat, norm_g[i, 1], m_lat[3], m_lat[4]), moe_router[i], moe_w1[i], moe_w3[i], moe_w2[i])
        if need_ctx:
            x_ctx = x_ctx + m_ctx[2] * o_ctx
            x_ctx = x_ctx + m_ctx[5] * expert_choice_ffn(
                modulate(x_ctx, norm_g[i, 1], m_ctx[3], m_ctx[4]), moe_router[i], moe_w1[i], moe_w3[i], moe_w2[i])
    return rms_norm(x_lat, final_g)
```

```python
import math
import numpy as np
import ml_dtypes
import concourse.bass as bass
import concourse.mybir as mybir
from concourse.bass_utils import run_bass_kernel_spmd

F32 = mybir.dt.float32
BF16 = mybir.dt.bfloat16
AF = mybir.ActivationFunctionType
ALU = mybir.AluOpType
AX = mybir.AxisListType
NPBF = ml_dtypes.bfloat16

ENGS = ("pe", "act", "dve", "pool", "sp")
N_DMA_SEMS = 6
NCORES = 8


class Prog:
    def __init__(self):
        self.nc = bass.Bass("TRN2", target_bir_lowering=False)
        self.ops = {e: [] for e in ENGS}
        self.cnt = {e: 0 for e in ENGS}
        self.dma_cnt = {e: [0] * N_DMA_SEMS for e in ENGS}
        self.dma_rr = {e: 0 for e in ENGS}
        self.last_w = {}
        self.readers = {}
        self.seen = {e: {} for e in ENGS}
        self._ctx = []
        self.n_inst = 0
        self._uid = 0

    def sb(self, name, shape, dt=F32):
        g = self.nc.sbuf_tensor(name, list(shape), dt)
        t = g.__enter__()
        self._ctx.append(g)
        return t

    def ps(self, name, shape, dt=F32):
        g = self.nc.psum_tensor(name, list(shape), dt)
        t = g.__enter__()
        self._ctx.append(g)
        return t

    def dram(self, name, shape, dt=F32, kind="ExternalInput"):
        if kind is None:
            return self.nc.dram_tensor(name, list(shape), dt).ap()
        return self.nc.dram_tensor(name, list(shape), dt, kind=kind).ap()

    @staticmethod
    def _key(x):
        if isinstance(x, (str, tuple)):
            return x
        t = getattr(x, "tensor", x)
        return getattr(t, "name", None) or str(t)

    def _deps(self, reads, writes):
        toks = []
        for r in reads:
            k = self._key(r)
            if k in self.last_w:
                toks.append(self.last_w[k])
        for w in writes:
            k = self._key(w)
            if k in self.last_w:
                toks.append(self.last_w[k])
            toks.extend(self.readers.get(k, ()))
        return toks

    def _commit(self, tok, reads, writes):
        for r in reads:
            self.readers.setdefault(self._key(r), []).append(tok)
        for w in writes:
            k = self._key(w)
            self.last_w[k] = tok
            self.readers[k] = []

    def _waits(self, eng, toks):
        need = {}
        for (sname, val) in toks:
            if val > need.get(sname, 0):
                need[sname] = val
        out = []
        seen = self.seen[eng]
        for sname, val in need.items():
            if seen.get(sname, 0) >= val:
                continue
            seen[sname] = val
            out.append((sname, val))
        return out

    def op(self, eng, fn, reads=(), writes=()):
        toks = self._deps(reads, writes)
        waits = self._waits(eng, toks)
        self.cnt[eng] += 1
        tok = ("c_" + eng, self.cnt[eng])
        self.ops[eng].append((waits, fn, ("c_" + eng, 1)))
        self._commit(tok, reads, writes)
        self.n_inst += 1
        return tok

    def dma(self, eng, out, in_, reads=None, writes=None, **kw):
        reads = [in_] if reads is None else reads
        writes = [out] if writes is None else writes
        toks = self._deps(reads, writes)
        i = self.dma_rr[eng]
        self.dma_rr[eng] = (i + 1) % N_DMA_SEMS
        sname = "d_%s_%d" % (eng, i)
        prev = self.dma_cnt[eng][i]
        if prev > 0:
            toks.append((sname, 16 * prev))
        waits = self._waits(eng, toks)
        self.dma_cnt[eng][i] = prev + 1
        tok = (sname, 16 * (prev + 1))

        def fn(e, out=out, in_=in_, kw=kw):
            return e.dma_start(out=out, in_=in_, **kw)

        self.ops[eng].append((waits, fn, (sname, 16)))
        self._commit(tok, reads, writes)
        self.n_inst += 1
        return tok

    def build(self):
        nc = self.nc
        toks = []
        for e in ENGS:
            if self.cnt[e]:
                toks.append(("c_" + e, self.cnt[e]))
            for i in range(N_DMA_SEMS):
                if self.dma_cnt[e][i]:
                    toks.append(("d_%s_%d" % (e, i), 16 * self.dma_cnt[e][i]))
        self.ops["sp"].append((self._waits("sp", toks), None, None))
        names = set()
        for e in ENGS:
            for waits, fn, inc in self.ops[e]:
                for s, _ in waits:
                    names.add(s)
                if inc:
                    names.add(inc[0])
        sems = {}
        for s in sorted(names):
            g = nc.semaphore(s)
            sems[s] = g.__enter__()
            self._ctx.append(g)
        blk = nc.Block()
        block = blk.__enter__()
        emap = {"pe": block.tensor, "act": block.scalar, "dve": block.vector,
                "pool": block.gpsimd, "sp": block.sync}
        for e in ENGS:
            lst = self.ops[e]
            if not lst:
                continue

            def body(eng, lst=lst):
                for waits, fn, inc in lst:
                    for s, v in waits:
                        eng.wait_ge(sems[s], v)
                    if fn is not None:
                        fn(eng).then_inc(sems[inc[0]], inc[1])

            emap[e](body)
        blk.__exit__(None, None, None)
        for g in reversed(self._ctx):
            g.__exit__(None, None, None)
        return nc

    def copy(self, eng, out, in_):
        if eng == "act":
            return self.op("act", lambda e: e.copy(out=out, in_=in_), [in_], [out])
        return self.op(eng, lambda e: e.tensor_copy(out=out, in_=in_), [in_], [out])

    def tt(self, eng, out, a, b, op):
        return self.op(eng, lambda e: e.tensor_tensor(out=out, in0=a, in1=b, op=op), [a, b], [out])

    def ts(self, eng, out, a, s1, op0, s2=None, op1=None, accum=None):
        rd = [a] + [s for s in (s1, s2) if not isinstance(s, (int, float, type(None)))]
        wr = [out] + ([accum] if accum is not None else [])
        kw = {}
        if op1 is not None:
            kw["op1"] = op1
        if accum is not None:
            kw["accum_out"] = accum
        return self.op(eng, lambda e: e.tensor_scalar(out=out, in0=a, scalar1=s1, scalar2=s2, op0=op0, **kw), rd, wr)

    def stt(self, eng, out, a, s, b, op0, op1):
        rd = [a, b] + ([s] if not isinstance(s, (int, float)) else [])
        return self.op(eng, lambda e: e.scalar_tensor_tensor(out=out, in0=a, scalar=s, in1=b, op0=op0, op1=op1), rd, [out])

    def actf(self, out, in_, func, bias=None, scale=None, accum=None):
        rd = [in_] + [s for s in (bias, scale) if s is not None and not isinstance(s, (int, float))]
        wr = [out] + ([accum] if accum is not None else [])
        kw = {}
        if bias is not None:
            kw["bias"] = bias
        if scale is not None:
            kw["scale"] = scale
        if accum is not None:
            kw["accum_out"] = accum
        return self.op("act", lambda e: e.activation(out=out, in_=in_, func=func, **kw), rd, wr)

    def mm(self, out, lhsT, rhs, start=True, stop=True):
        return self.op("pe", lambda e: e.matmul(out, lhsT, rhs, start=start, stop=stop), [lhsT, rhs], [out])

    def tr(self, out, in_, ident):
        return self.op("pe", lambda e: e.transpose(out, in_, ident), [in_, ident], [out])


_PROG_CACHE = {}
N_LAUNCH = [0]


def launch(key, builder, in_maps):
    if key not in _PROG_CACHE:
        P = builder()
        P.build()
        _PROG_CACHE[key] = P
    P = _PROG_CACHE[key]
    N_LAUNCH[0] += 1
    res = run_bass_kernel_spmd(P.nc, in_maps, core_ids=list(range(NCORES)))
    return res.results


D = 1024
B = 4
SEQ = 4096
CTXL = 256
NTOK = B * SEQ + B * CTXL
TPC = NTOK // NCORES
NT = TPC // 128
EPS = 1e-6


def tile_modrow(gt):
    return gt // 32 if gt < 128 else 4


def build_mod():
    P = Prog()
    cT = P.dram("cT", [D, 5])
    w = P.dram("w", [D, 3072])
    bias = P.dram("bias", [1, 3072])
    out = P.dram("out", [5, 3072], kind="ExternalOutput")
    ct = P.sb("ct", [128, 8, 5])
    sg = P.sb("sg", [128, 8, 5])
    st = P.sb("st", [128, 8, 5])
    bt = P.sb("bt", [5, 3072])
    ot = P.sb("ot", [5, 3072])
    wts = [P.sb("wt%d" % i, [128, 3072]) for i in range(8)]
    accs = [P.ps("acc%d" % i, [128, 512]) for i in range(6)]
    P.dma("sp", ct[:], cT.rearrange("(k p) r -> p k r", p=128))
    P.dma("act", bt[:], bias.partition_broadcast(5))
    for k in range(8):
        P.dma("sp" if k % 2 == 0 else "act", wts[k][:], w[k * 128:(k + 1) * 128, :])
    P.actf(sg[:], ct[:], AF.Sigmoid)
    P.tt("dve", st[:], ct[:], sg[:], ALU.mult)
    for n in range(6):
        for k in range(8):
            P.mm(accs[n][0:5, :], st[:, k, :], wts[k][:, n * 512:(n + 1) * 512], start=(k == 0), stop=(k == 7))
        P.tt("dve", ot[:, n * 512:(n + 1) * 512], accs[n][0:5, :], bt[:, n * 512:(n + 1) * 512], ALU.add)
    P.dma("sp", out, ot[:])
    return P


def run_mod(c, c_ctx, ada_w, ada_b):
    cT = np.ascontiguousarray(np.concatenate([c, c_ctx[None]], 0).T)
    maps = []
    for core in range(NCORES):
        i, hf = core // 2, core % 2
        maps.append({"cT": cT, "w": np.ascontiguousarray(ada_w[i][:, hf * 3072:(hf + 1) * 3072]),
                     "bias": np.ascontiguousarray(ada_b[i][None, hf * 3072:(hf + 1) * 3072])})
    res = launch("mod", build_mod, maps)
    mods = np.zeros((4, 5, 6144), np.float32)
    for core in range(NCORES):
        i, hf = core // 2, core % 2
        mods[i][:, hf * 3072:(hf + 1) * 3072] = res[core]["out"]
    return mods


def build_tok_a(ncols, combine, rope, final=False, pbf=False):
    P = Prog()
    x1 = P.dram("x1", [TPC, D])
    if combine:
        ya = P.dram("ya", [TPC, D])
        gm = P.dram("gm", [NT, D])
        xo = P.dram("xo", [TPC, D], kind="ExternalOutput")
    msh = P.dram("msh", [NT, D])
    msc = P.dram("msc", [NT, D])
    g = P.dram("g", [1, D])
    w = None if final else P.dram("w", [D, ncols])
    ident_d = P.dram("ident", [128, 128], BF16)
    if rope:
        rc = P.dram("rc", [TPC, 64])
        rs = P.dram("rs", [TPC, 64])
    pout = P.dram("p", [TPC, ncols], BF16 if pbf else F32, kind="ExternalOutput")

    ident = P.sb("identb", [128, 128], BF16)
    P.dma("sp", ident[:], ident_d)
    gbc = P.sb("gbc", [128, D])
    P.dma("sp", gbc[:], g.partition_broadcast(128))
    wb = P.sb("wb", [128, 8, ncols], BF16)
    wst = [P.sb("wst%d" % i, [128, ncols]) for i in range(2)]
    for k in range(0 if final else 8):
        P.dma("sp" if k % 2 == 0 else "act", wst[k % 2][:], w[k * 128:(k + 1) * 128, :])
        P.copy("pool" if k % 2 == 0 else "dve", wb[:, k, :], wst[k % 2][:])

    xts = [P.sb("xt%d" % i, [128, D]) for i in range(2)]
    if combine:
        yas = [P.sb("ya%d" % i, [128, D]) for i in range(2)]
        gms = [P.sb("gm%d" % i, [128, D]) for i in range(2)]
    scs = [P.sb("sc%d" % i, [128, D]) for i in range(2)]
    shs = [P.sb("sh%d" % i, [128, D]) for i in range(2)]
    junk = P.sb("junk", [128, D])
    ssq = [P.sb("ssq%d" % i, [128, 1]) for i in range(2)]
    rstd = [P.sb("rstd%d" % i, [128, 1]) for i in range(2)]
    h32 = P.sb("h32", [128, D])
    hb = [P.sb("hb%d" % i, [128, D], BF16) for i in range(2)]
    hT = [P.sb("hT%d" % i, [128, D], BF16) for i in range(2)]
    tp = [P.ps("tp%d" % i, [128, D], BF16) for i in range(2)]
    accs = [P.ps("acc%d" % i, [128, 512]) for i in range(4)]
    pts = [P.sb("pt%d" % i, [128, ncols]) for i in range(2)]
    if pbf:
        ptb = [P.sb("ptb%d" % i, [128, ncols], BF16) for i in range(2)]
    if rope:
        rcs = [P.sb("rc%d" % i, [128, 64]) for i in range(2)]
        rss = [P.sb("rs%d" % i, [128, 64]) for i in range(2)]
        t1 = P.sb("t1", [128, 2048])
        t2 = P.sb("t2", [128, 2048])
    nchunks = [(n0, min(512, ncols - n0)) for n0 in range(0, ncols, 512)]
    ai = 0
    for t in range(NT):
        b2 = t % 2
        rows = slice(t * 128, (t + 1) * 128)
        xt = xts[b2]
        P.dma("sp", xt[:], x1[rows, :])
        P.dma("sp", scs[b2][:], msc[t:t + 1, :].partition_broadcast(128))
        P.dma("sp", shs[b2][:], msh[t:t + 1, :].partition_broadcast(128))
        if combine:
            P.dma("act", yas[b2][:], ya[rows, :])
            P.dma("act", gms[b2][:], gm[t:t + 1, :].partition_broadcast(128))
            P.tt("pool", yas[b2][:], yas[b2][:], gms[b2][:], ALU.mult)
            P.tt("dve", xt[:], xt[:], yas[b2][:], ALU.add)
            P.dma("pool", xo[rows, :], xt[:])
        if rope:
            P.dma("act", rcs[b2][:], rc[rows, :])
            P.dma("act", rss[b2][:], rs[rows, :])
        P.actf(junk[:], xt[:], AF.Square, accum=ssq[b2][:])
        P.ts("dve", rstd[b2][:], ssq[b2][:], 1.0 / D, ALU.mult, EPS, ALU.add)
        P.actf(rstd[b2][:], rstd[b2][:], AF.Sqrt)
        P.op("dve", lambda e, o=rstd[b2]: e.reciprocal(out=o[:], in_=o[:]), [rstd[b2]], [rstd[b2]])
        P.ts("pool", scs[b2][:], scs[b2][:], 1.0, ALU.add)
        P.tt("pool", scs[b2][:], scs[b2][:], gbc[:], ALU.mult)
        P.stt("dve", h32[:], xt[:], rstd[b2][:], scs[b2][:], ALU.mult, ALU.mult)
        if final:
            P.tt("pool", h32[:], h32[:], shs[b2][:], ALU.add)
            P.dma("pool", pout[rows, :], h32[:])
            continue
        P.tt("pool", hb[b2][:], h32[:], shs[b2][:], ALU.add)
        for k in range(8):
            P.tr(tp[b2][:, k * 128:(k + 1) * 128], hb[b2][:, k * 128:(k + 1) * 128], ident[:])
        P.copy("act", hT[b2][:], tp[b2][:])
        pt = pts[b2]
        for ci, (n0, nw) in enumerate(nchunks):
            acc = accs[ai % 4]
            ai += 1
            for k in range(8):
                P.mm(acc[:, 0:nw], hT[b2][:, k * 128:(k + 1) * 128], wb[:, k, n0:n0 + nw], start=(k == 0), stop=(k == 7))
            P.copy("act" if ci % 2 == 0 else "dve", pt[:, n0:n0 + nw], acc[:, 0:nw])
        if rope:
            qk = pt[:, 0:2048]
            c64 = rcs[b2][:].unsqueeze(1).broadcast_to([128, 32, 64])
            P.tt("dve", t1[:].rearrange("p (n d) -> p n d", d=64), qk.rearrange("p (n d) -> p n d", d=64), c64, ALU.mult)
            xv = qk.rearrange("p (n a h f) -> p n a h f", a=2, h=2, f=16)
            tv = t2[:].rearrange("p (n a h f) -> p n a h f", a=2, h=2, f=16)
            sv = rss[b2][:].rearrange("p (a h f) -> p a h f", a=2, h=2)
            for hh in range(2):
                s_b = sv[:, :, hh, :].unsqueeze(1).broadcast_to([128, 32, 2, 16])
                P.tt("pool", tv[:, :, :, hh, :], xv[:, :, :, 1 - hh, :], s_b, ALU.mult)
            P.tt("dve", qk, t1[:], t2[:], ALU.add)
        if pbf:
            P.copy("act", ptb[b2][:], pt[:])
            P.dma("pool", pout[rows, :], ptb[b2][:])
        else:
            P.dma("pool", pout[rows, :], pt[:])
    return P


def rope_tables():
    L = SEQ
    row = np.repeat(np.arange(L // 64, dtype=np.float32), 64)
    col = np.tile(np.arange(64, dtype=np.float32), L // 64)
    inv = (10000.0 ** (-np.arange(16, dtype=np.float32) / 16)).astype(np.float32)
    ar = row[:, None] * inv
    ac = col[:, None] * inv
    C = np.concatenate([np.cos(ar), np.cos(ar), np.cos(ac), np.cos(ac)], 1).astype(np.float32)
    S = np.concatenate([-np.sin(ar), np.sin(ar), -np.sin(ac), np.sin(ac)], 1).astype(np.float32)
    Cf = np.concatenate([np.tile(C, (B, 1)), np.ones((B * CTXL, 64), np.float32)], 0)
    Sf = np.concatenate([np.tile(S, (B, 1)), np.zeros((B * CTXL, 64), np.float32)], 0)
    return Cf, Sf


def modrows(mods_i, k):
    out = []
    for core in range(NCORES):
        rows = [tile_modrow(core * NT + t) for t in range(NT)]
        out.append(np.ascontiguousarray(mods_i[rows, k * D:(k + 1) * D]))
    return out


IDENT = np.eye(128, dtype=np.float32).astype(NPBF)


def run_tok_a(x1, mods_i, g, w, prev=None, rope=False, final=False, pbf=False):
    ncols = D if final else w.shape[1]
    combine = prev is not None
    msh, msc = modrows(mods_i, 0), modrows(mods_i, 1)
    if combine:
        gm = modrows(prev[1], 5)
    if rope:
        Cf, Sf = rope_tables()
    maps = []
    for core in range(NCORES):
        r = slice(core * TPC, (core + 1) * TPC)
        m = {"x1": x1[r], "msh": msh[core], "msc": msc[core], "g": g[None, :], "ident": IDENT}
        if not final:
            m["w"] = w
        if combine:
            m.update(ya=prev[0][r], gm=gm[core])
        if rope:
            m.update(rc=Cf[r], rs=Sf[r])
        maps.append(m)
    res = launch(("tok_a", ncols, combine, rope, final, pbf), lambda: build_tok_a(ncols, combine, rope, final, pbf), maps)
    p = np.concatenate([res[c]["p"] for c in range(NCORES)], 0)
    xo = np.concatenate([res[c]["xo"] for c in range(NCORES)], 0) if combine else x1
    return xo, p


def build_tok_c():
    P = Prog()
    mix = P.dram("mix", [TPC, D])
    x = P.dram("x", [TPC, D])
    mg = P.dram("mg", [NT, D])
    msh = P.dram("msh", [NT, D])
    msc = P.dram("msc", [NT, D])
    g = P.dram("g", [1, D])
    w = P.dram("w", [D, D])
    rw = P.dram("rw", [D, 16])
    ident_d = P.dram("ident", [128, 128], BF16)
    identf_d = P.dram("identf", [128, 128])
    x1o = P.dram("x1o", [TPC, D], kind="ExternalOutput")
    h2o = P.dram("h2o", [TPC, D], BF16, kind="ExternalOutput")
    affo = P.dram("affo", [TPC, 16], kind="ExternalOutput")

    ident = P.sb("identb", [128, 128], BF16)
    identf = P.sb("identfs", [128, 128])
    P.dma("sp", ident[:], ident_d)
    P.dma("sp", identf[:], identf_d)
    gbc = P.sb("gbc", [128, D])
    P.dma("sp", gbc[:], g.partition_broadcast(128))
    rwt = P.sb("rwt", [128, 8, 16])
    P.dma("sp", rwt[:], rw.rearrange("(k p) e -> p k e", p=128))
    wb = P.sb("wb", [128, 8, D], BF16)
    wst = [P.sb("wst%d" % i, [128, D]) for i in range(2)]
    for k in range(8):
        P.dma("sp" if k % 2 == 0 else "act", wst[k % 2][:], w[k * 128:(k + 1) * 128, :])
        P.copy("pool" if k % 2 == 0 else "dve", wb[:, k, :], wst[k % 2][:])

    mts = [P.sb("mt%d" % i, [128, D]) for i in range(2)]
    xts = [P.sb("xt%d" % i, [128, D]) for i in range(2)]
    mgs = [P.sb("mg%d" % i, [128, D]) for i in range(2)]
    scs = [P.sb("sc%d" % i, [128, D]) for i in range(2)]
    shs = [P.sb("sh%d" % i, [128, D]) for i in range(2)]
    mb = [P.sb("mb%d" % i, [128, D], BF16) for i in range(2)]
    mT = [P.sb("mT%d" % i, [128, D], BF16) for i in range(2)]
    junk = P.sb("junk", [128, D])
    ssq = [P.sb("ssq%d" % i, [128, 1]) for i in range(2)]
    rstd = [P.sb("rstd%d" % i, [128, 1]) for i in range(2)]
    h32 = [P.sb("h32_%d" % i, [128, D]) for i in range(2)]
    hb = [P.sb("hb%d" % i, [128, D], BF16) for i in range(2)]
    hTf = [P.sb("hTf%d" % i, [128, D]) for i in range(2)]
    tp = [P.ps("tp%d" % i, [128, D], BF16) for i in range(2)]
    tpf = [P.ps("tpf%d" % i, [128, 512]) for i in range(2)]
    accs = [P.ps("acc%d" % i, [128, 512]) for i in range(2)]
    lg = P.ps("lg", [128, 16])
    lgs = [P.sb("lgs%d" % i, [128, 16]) for i in range(2)]
    mx = [P.sb("mx%d" % i, [128, 1]) for i in range(2)]
    sm = [P.sb("sm%d" % i, [128, 1]) for i in range(2)]
    for t in range(NT):
        b2 = t % 2
        rows = slice(t * 128, (t + 1) * 128)
        P.dma("sp", mts[b2][:], mix[rows, :])
        P.dma("sp", xts[b2][:], x[rows, :])
        P.dma("act", mgs[b2][:], mg[t:t + 1, :].partition_broadcast(128))
        P.dma("act", scs[b2][:], msc[t:t + 1, :].partition_broadcast(128))
        P.dma("act", shs[b2][:], msh[t:t + 1, :].partition_broadcast(128))
        P.copy("pool", mb[b2][:], mts[b2][:])
        for k in range(8):
            P.tr(tp[b2][:, k * 128:(k + 1) * 128], mb[b2][:, k * 128:(k + 1) * 128], ident[:])
        P.copy("act", mT[b2][:], tp[b2][:])
        xt = xts[b2]
        for n in range(2):
            acc = accs[n]
            for k in range(8):
                P.mm(acc[:], mT[b2][:, k * 128:(k + 1) * 128], wb[:, k, n * 512:(n + 1) * 512], start=(k == 0), stop=(k == 7))
            sl = slice(n * 512, (n + 1) * 512)
            P.tt("dve", mts[b2][:, sl], acc[:], mgs[b2][:, sl], ALU.mult)
        P.tt("pool", xt[:], xt[:], mts[b2][:], ALU.add)
        P.dma("pool", x1o[rows, :], xt[:])
        P.actf(junk[:], xt[:], AF.Square, accum=ssq[b2][:])
        P.ts("dve", rstd[b2][:], ssq[b2][:], 1.0 / D, ALU.mult, EPS, ALU.add)
        P.actf(rstd[b2][:], rstd[b2][:], AF.Sqrt)
        P.op("dve", lambda e, o=rstd[b2]: e.reciprocal(out=o[:], in_=o[:]), [rstd[b2]], [rstd[b2]])
        P.ts("pool", scs[b2][:], scs[b2][:], 1.0, ALU.add)
        P.tt("pool", scs[b2][:], scs[b2][:], gbc[:], ALU.mult)
        P.stt("dve", h32[b2][:], xt[:], rstd[b2][:], scs[b2][:], ALU.mult, ALU.mult)
        P.tt("pool", h32[b2][:], h32[b2][:], shs[b2][:], ALU.add)
        P.copy("act", hb[b2][:], h32[b2][:])
        P.dma("pool", h2o[rows, :], hb[b2][:])
        for half in range(2):
            for k in range(4):
                kk = half * 4 + k
                P.tr(tpf[half][:, k * 128:(k + 1) * 128], h32[b2][:, kk * 128:(kk + 1) * 128], identf[:])
            P.copy("act" if half == 0 else "dve", hTf[b2][:, half * 512:(half + 1) * 512], tpf[half][:])
        for k in range(8):
            P.mm(lg[:], hTf[b2][:, k * 128:(k + 1) * 128], rwt[:, k, :], start=(k == 0), stop=(k == 7))
        P.op("dve", lambda e, o=mx[b2]: e.reduce_max(out=o[:], in_=lg[:], axis=AX.X), [lg], [mx[b2]])
        P.ts("dve", mx[b2][:], mx[b2][:], -1.0, ALU.mult)
        P.actf(lgs[b2][:], lg[:], AF.Exp, bias=mx[b2][:], accum=sm[b2][:])
        P.op("dve", lambda e, o=sm[b2]: e.reciprocal(out=o[:], in_=o[:]), [sm[b2]], [sm[b2]])
        P.ts("dve", lgs[b2][:], lgs[b2][:], sm[b2][:], ALU.mult)
        P.dma("pool", affo[rows, :], lgs[b2][:])
    return P


IDENTF = np.eye(128, dtype=np.float32)


def run_tok_c(mix, x, mods_i, g2, w_out, router):
    mg, msh, msc = modrows(mods_i, 2), modrows(mods_i, 3), modrows(mods_i, 4)
    maps = []
    for core in range(NCORES):
        r = slice(core * TPC, (core + 1) * TPC)
        maps.append({"mix": mix[r], "x": x[r], "mg": mg[core], "msh": msh[core], "msc": msc[core],
                     "g": g2[None, :], "w": w_out, "rw": router, "ident": IDENT, "identf": IDENTF})
    res = launch("tok_c", build_tok_c, maps)
    cat = lambda k: np.concatenate([res[c][k] for c in range(NCORES)], 0)
    return cat("x1o"), cat("h2o"), cat("affo")


def build_topk():
    P = Prog()
    aff = P.dram("aff", [8, SEQ + CTXL])
    out = P.dram("gt", [8, SEQ + CTXL], kind="ExternalOutput")
    a = P.sb("a", [8, SEQ + CTXL])
    wk = P.sb("wk", [8, SEQ + CTXL])
    m8 = P.sb("m8", [8, 8])
    P.dma("sp", a[:], aff)
    P.copy("dve", wk[:], a[:])
    for (lo, n, cap) in ((0, SEQ, 2 * SEQ // 16), (SEQ, CTXL, 2 * CTXL // 16)):
        seg = wk[:, lo:lo + n]
        for r in range(cap // 8):
            P.op("dve", lambda e, seg=seg: e.max(out=m8[:], in_=seg), [wk], [m8])
            P.op("dve", lambda e, seg=seg: e.match_replace(out=seg, in_to_replace=m8[:], in_values=seg, imm_value=0.0), [wk, m8], [wk])
    P.tt("dve", wk[:], a[:], wk[:], ALU.subtract)
    P.dma("sp", out, wk[:])
    return P


def run_topk(aff):
    al = aff[:B * SEQ].reshape(B, SEQ, 16)
    ac = aff[B * SEQ:].reshape(B, CTXL, 16)
    maps = []
    for core in range(NCORES):
        b, hf = core // 2, core % 2
        at = np.concatenate([al[b, :, hf * 8:(hf + 1) * 8].T, ac[b, :, hf * 8:(hf + 1) * 8].T], 1)
        maps.append({"aff": np.ascontiguousarray(at)})
    res = launch("topk", build_topk, maps)
    G = np.zeros((NTOK, 16), np.float32)
    for core in range(NCORES):
        b, hf = core // 2, core % 2
        gt = res[core]["gt"]
        G[b * SEQ:(b + 1) * SEQ, hf * 8:(hf + 1) * 8] = gt[:, :SEQ].T
        G[B * SEQ + b * CTXL:B * SEQ + (b + 1) * CTXL, hf * 8:(hf + 1) * 8] = gt[:, SEQ:].T
    return G


def build_moe():
    P = Prog()
    hT = P.dram("hT", [D, TPC], BF16)
    G = P.dram("G", [TPC, 16])
    w1 = P.dram("w1", [16, D, D])
    w3 = P.dram("w3", [16, D, D])
    w2 = P.dram("w2", [16, D, D])
    yo = P.dram("y", [TPC, D], kind="ExternalOutput")
    hs = P.sb("hs", [128, 8, TPC], BF16)
    for k in range(8):
        P.dma("sp" if k % 2 == 0 else "act", hs[:, k, :], hT[k * 128:(k + 1) * 128, :])
    Gs = P.sb("Gs", [128, NT, 16])
    P.dma("sp", Gs[:], G.rearrange("(t p) e -> p t e", p=128))
    yacc = P.sb("yacc", [128, NT, D])
    wb = {nm: P.sb(nm + "b", [128, 8, D], BF16) for nm in ("w1", "w3", "w2")}
    wsrc = {"w1": w1, "w3": w3, "w2": w2}
    wst = [P.sb("wst%d" % i, [128, D]) for i in range(4)]
    hact = P.sb("hact", [128, 8, 512], BF16)
    sA = [P.sb("sA%d" % i, [128, 512]) for i in range(2)]
    pb = [P.ps("pb%d" % i, [128, 512]) for i in range(8)]
    chunks = [(c0, min(512, TPC - c0)) for c0 in range(0, TPC, 512)]
    si = 0
    cast_engs = ("dve", "pool", "act")
    for e in range(16):
        for nm in ("w1", "w3", "w2"):
            for k in range(8):
                st = wst[si % 4]
                P.dma("sp" if si % 2 == 0 else "act", st[:], wsrc[nm][e, k * 128:(k + 1) * 128, :])
                P.copy(cast_engs[si % 3], wb[nm][:, k, :], st[:])
                si += 1
        for (c0, cn) in chunks:
            for fc in range(8):
                A = pb[(fc % 2) * 2]
                Bm = pb[(fc % 2) * 2 + 1]
                for kd in range(8):
                    P.mm(A[:, 0:cn], wb["w1"][:, kd, fc * 128:(fc + 1) * 128], hs[:, kd, c0:c0 + cn], start=(kd == 0), stop=(kd == 7))
                for kd in range(8):
                    P.mm(Bm[:, 0:cn], wb["w3"][:, kd, fc * 128:(fc + 1) * 128], hs[:, kd, c0:c0 + cn], start=(kd == 0), stop=(kd == 7))
                P.actf(sA[fc % 2][:, 0:cn], A[:, 0:cn], AF.Silu)
                P.tt("dve", hact[:, fc, 0:cn], sA[fc % 2][:, 0:cn], Bm[:, 0:cn], ALU.mult)
            for tt in range(cn // 128):
                tile = c0 // 128 + tt
                for n2 in range(2):
                    acc = pb[4 + (tt * 2 + n2) % 4]
                    for fc in range(8):
                        P.mm(acc[:], hact[:, fc, tt * 128:(tt + 1) * 128], wb["w2"][:, fc, n2 * 512:(n2 + 1) * 512], start=(fc == 0), stop=(fc == 7))
                    ysl = yacc[:, tile, n2 * 512:(n2 + 1) * 512]
                    if e == 0:
                        P.ts("dve", ysl, acc[:], Gs[:, tile, e:e + 1], ALU.mult)
                    else:
                        P.stt("dve", ysl, acc[:], Gs[:, tile, e:e + 1], ysl, ALU.mult, ALU.add)
    for t in range(NT):
        P.dma("sp" if t % 2 == 0 else "pool", yo[t * 128:(t + 1) * 128, :], yacc[:, t, :])
    return P


def run_moe(h2, G, w1, w3, w2):
    maps = []
    for core in range(NCORES):
        r = slice(core * TPC, (core + 1) * TPC)
        maps.append({"hT": np.ascontiguousarray(h2[r].T), "G": G[r], "w1": w1, "w3": w3, "w2": w2})
    res = launch("moe", build_moe, maps)
    return np.concatenate([res[c]["y"] for c in range(NCORES)], 0)


NKEY = SEQ + CTXL
NQ = SEQ // 2


def build_attn(lam_init, NQ=SEQ // 2, NKEY=SEQ + CTXL):
    P = Prog()
    QB = min(512, NQ)
    NQT = QB // 128
    NKC = NKEY // 128
    qT = P.dram("qT", [16, 64, NQ], BF16)
    kT = P.dram("kT", [16, 64, NKEY], BF16)
    va = P.dram("va", [8, NKEY, 132], BF16)
    lamv = P.dram("lamv", [1, 256])
    sg = P.dram("sg", [1, 128])
    out = P.dram("o", [NQ, 8 * 128], kind="ExternalOutput")
    lt = P.sb("lt", [128, 256])
    P.dma("sp", lt[:], lamv.partition_broadcast(128))
    sgb = P.sb("sgb", [128, 128])
    P.dma("sp", sgb[:], sg.partition_broadcast(128))
    P.ts("dve", sgb[:], sgb[:], 1.0 - lam_init, ALU.mult)
    lp = P.sb("lp", [128, 128])
    ls = P.sb("ls", [128, 2])
    lv = lt[:].rearrange("p (a d) -> p a d", a=4)
    P.tt("dve", lp[:, 0:64], lv[:, 0, :], lv[:, 1, :], ALU.mult)
    P.tt("dve", lp[:, 64:128], lv[:, 2, :], lv[:, 3, :], ALU.mult)
    P.op("dve", lambda e: e.reduce_sum(out=ls[:], in_=lp[:].rearrange("p (a d) -> p a d", a=2), axis=AX.X), [lp], [ls])
    P.actf(ls[:], ls[:], AF.Exp)
    nlam = P.sb("nlam", [128, 1])
    P.tt("dve", nlam[:], ls[:, 1:2], ls[:, 0:1], ALU.subtract)
    P.ts("dve", nlam[:], nlam[:], -lam_init, ALU.add)

    qs = [P.sb("qs%d" % i, [64, 2, NQ], BF16) for i in range(2)]
    ks = [P.sb("ks%d" % i, [64, 2, NKEY], BF16) for i in range(2)]
    vs = [P.sb("vs%d" % i, [128, NKC, 132], BF16) for i in range(2)]
    sps = [P.ps("sp%d" % i, [128, 512]) for i in range(2)]
    ops_ = [P.ps("op%d" % i, [128, 512]) for i in range(4)]
    es = [P.sb("es%d" % i, [128, 512], BF16) for i in range(3)]
    o0 = [P.sb("o0_%d" % i, [128, 4, 128]) for i in range(2)]
    rz = [P.sb("rz%d" % i, [128, 4]) for i in range(2)]
    o1 = P.sb("o1", [128, 128])
    junk = P.sb("junk", [128, 128])
    ss = P.sb("ss", [128, 1])
    ob = [P.sb("ob%d" % i, [128, 8 * 128]) for i in range(4)]
    ei = 0
    si = 0
    for qb in range(NQ // QB):
        for h in range(8):
            hb_ = (qb * 8 + h) % 2
            P.dma("sp", qs[hb_][:], qT[2 * h:2 * h + 2, :, :].rearrange("c d q -> d c q"))
            P.dma("act", ks[hb_][:], kT[2 * h:2 * h + 2, :, :].rearrange("c d k -> d c k"))
            P.dma("sp", vs[hb_][:], va[h].rearrange("(n p) e -> p n e", p=128))
            for c in range(2):
                for kc in range(NKC):
                    spb = sps[si % 2]
                    si += 1
                    P.mm(spb[:, 0:QB], ks[hb_][:, c, kc * 128:(kc + 1) * 128], qs[hb_][:, c, qb * QB:(qb + 1) * QB])
                    eb = es[ei % 3]
                    ei += 1
                    P.actf(eb[:, 0:QB], spb[:, 0:QB], AF.Exp, scale=0.125)
                    for qt in range(NQT):
                        P.mm(ops_[qt][:, 0:132], eb[:, qt * 128:(qt + 1) * 128], vs[hb_][:, kc, :], start=(kc == 0), stop=(kc == NKC - 1))
                for qt in range(NQT):
                    if c == 0:
                        P.op("dve", lambda e, qt=qt, r=rz[0]: e.reciprocal(out=r[:, qt:qt + 1], in_=ops_[qt][:, 128:129]), [ops_[qt]], [rz[0]])
                        P.ts("dve", o0[0][:, qt, :], ops_[qt][:, 0:128], rz[0][:, qt:qt + 1], ALU.mult)
                    else:
                        P.op("dve", lambda e, qt=qt, r=rz[1]: e.reciprocal(out=r[:, qt:qt + 1], in_=ops_[qt][:, 128:129]), [ops_[qt]], [rz[1]])
                        P.ts("dve", o1[:], ops_[qt][:, 0:128], rz[1][:, qt:qt + 1], ALU.mult)
                        P.stt("dve", o1[:], o1[:], nlam[:], o0[0][:, qt, :], ALU.mult, ALU.add)
                        P.actf(junk[:], o1[:], AF.Square, accum=ss[:])
                        P.ts("dve", ss[:], ss[:], 1.0 / 128, ALU.mult, 1e-5, ALU.add)
                        P.actf(ss[:], ss[:], AF.Sqrt)
                        P.op("dve", lambda e: e.reciprocal(out=ss[:], in_=ss[:]), [ss], [ss])
                        P.stt("dve", ob[qt][:, h * 128:(h + 1) * 128], o1[:], ss[:], sgb[:], ALU.mult, ALU.mult)
        for qt in range(NQT):
            r0 = qb * QB + qt * 128
            P.dma("pool", out[r0:r0 + 128, :], ob[qt][:])
    return P


def run_attn(p, da_lambda, da_subln, lam_init):
    pl = p[:B * SEQ].reshape(B, SEQ, 3072)
    pc = p[B * SEQ:].reshape(B, CTXL, 3072)
    maps = []
    for core in range(NCORES):
        b, hf = core // 2, core % 2
        q = pl[b, hf * NQ:(hf + 1) * NQ, 0:1024].reshape(NQ, 16, 64)
        kk = np.concatenate([pc[b, :, 1024:2048], pl[b, :, 1024:2048]], 0).reshape(NKEY, 16, 64)
        v = np.concatenate([pc[b, :, 2048:3072], pl[b, :, 2048:3072]], 0).reshape(NKEY, 8, 128)
        va = np.zeros((8, NKEY, 132), NPBF)
        va[:, :, :128] = v.transpose(1, 0, 2)
        va[:, :, 128] = 1.0
        maps.append({"qT": np.ascontiguousarray(q.transpose(1, 2, 0)), "kT": np.ascontiguousarray(kk.transpose(1, 2, 0)),
                     "va": va, "lamv": np.ascontiguousarray(da_lambda.reshape(1, 256)), "sg": da_subln[None, :]})
    res = launch(("attn", lam_init), lambda: build_attn(lam_init), maps)
    o = np.zeros((NTOK, 1024), np.float32)
    for core in range(NCORES):
        b, hf = core // 2, core % 2
        o[b * SEQ + hf * NQ:b * SEQ + (hf + 1) * NQ] = res[core]["o"]
    return o


def run_attn_ctx(p, da_lambda, da_subln, lam_init):
    pc = p[B * SEQ:].reshape(B, CTXL, 3072)
    maps = []
    for core in range(NCORES):
        b, hf = core // 2, core % 2
        q = pc[b, hf * 128:(hf + 1) * 128, 0:1024].reshape(128, 16, 64)
        kk = pc[b, :, 1024:2048].reshape(CTXL, 16, 64)
        v = pc[b, :, 2048:3072].reshape(CTXL, 8, 128)
        va = np.zeros((8, CTXL, 132), NPBF)
        va[:, :, :128] = v.transpose(1, 0, 2)
        va[:, :, 128] = 1.0
        maps.append({"qT": np.ascontiguousarray(q.transpose(1, 2, 0)), "kT": np.ascontiguousarray(kk.transpose(1, 2, 0)),
                     "va": va, "lamv": np.ascontiguousarray(da_lambda.reshape(1, 256)), "sg": da_subln[None, :]})
    res = launch(("attn_ctx", lam_init), lambda: build_attn(lam_init, 128, CTXL), maps)
    o = np.zeros((B * CTXL, 1024), np.float32)
    for core in range(NCORES):
        b, hf = core // 2, core % 2
        o[b * CTXL + hf * 128:b * CTXL + (hf + 1) * 128] = res[core]["o"]
    return o


RWC = 1760
QC = 10 * 512 + 8


def seg_shift(p, k):
    out = np.zeros_like(p)
    segs = [(b * SEQ, SEQ) for b in range(B)] + [(B * SEQ + b * CTXL, CTXL) for b in range(B)]
    for (s0, n) in segs:
        if k == 1:
            out[s0 + 1:s0 + n] = p[s0:s0 + n - 1]
        else:
            out[s0:s0 + n - 1] = p[s0 + 1:s0 + n]
    return out


def build_rwprep():
    P = Prog()
    pc = P.dram("pc", [TPC, RWC])
    pp = P.dram("pp", [TPC, RWC])
    pn = P.dram("pn", [TPC, RWC])
    mu = P.dram("mu", [2, RWC])
    w0 = P.dram("w0", [2, 512])
    a0 = P.dram("a0", [2, 512])
    vec = P.dram("vec", [3, 512])
    lw_d = P.dram("lw", [128, 2048])
    lg_d = P.dram("lg", [128, 512])
    identf_d = P.dram("identf", [128, 128])
    qo = P.dram("q", [TPC, QC], kind="ExternalOutput")
    identf = P.sb("identfs", [128, 128])
    P.dma("sp", identf[:], identf_d)
    mub = [P.sb("mub%d" % i, [128, RWC]) for i in range(2)]
    w0b = [P.sb("w0b%d" % i, [128, 512]) for i in range(2)]
    a0b = [P.sb("a0b%d" % i, [128, 512]) for i in range(2)]
    vb = [P.sb("vb%d" % i, [128, 512]) for i in range(3)]
    for i in range(2):
        P.dma("sp", mub[i][:], mu[i:i + 1, :].partition_broadcast(128))
        P.dma("act", w0b[i][:], w0[i:i + 1, :].partition_broadcast(128))
        P.dma("act", a0b[i][:], a0[i:i + 1, :].partition_broadcast(128))
    for i in range(3):
        P.dma("sp", vb[i][:], vec[i:i + 1, :].partition_broadcast(128))
    lw = P.sb("lws", [128, 2048])
    lg = P.sb("lgs", [128, 512])
    P.dma("sp", lw[:], lw_d)
    P.dma("sp", lg[:], lg_d)
    X = P.sb("X", [128, 256])
    P.op("dve", lambda e: e.memset(X[:], 0.0), [], [X])
    pcs = [P.sb("pcs%d" % i, [128, RWC]) for i in range(2)]
    pps = [P.sb("pps%d" % i, [128, RWC]) for i in range(2)]
    pns = [P.sb("pns%d" % i, [128, RWC]) for i in range(2)]
    qt = [P.sb("qt%d" % i, [128, QC]) for i in range(2)]
    tpA = P.ps("tpA", [128, 256])
    XT = P.sb("XT", [128, 256])
    lin = [P.ps("lin%d" % i, [128, 512]) for i in range(5)]
    tw = [P.sb("tw%d" % i, [128, 512]) for i in range(2)]
    ad = [P.sb("ad%d" % i, [128, 512]) for i in range(2)]
    kr = P.sb("kr", [128, 512])
    sq = P.sb("sq", [128, 512])
    s8 = P.sb("s8", [128, 8])
    t5 = P.sb("t5", [128, 512])
    for t in range(NT):
        b2 = t % 2
        rows = slice(t * 128, (t + 1) * 128)
        u, up, un, q = pcs[b2], pps[b2], pns[b2], qt[b2]
        P.dma("sp", u[:], pc[rows, :])
        P.dma("act", up[:], pp[rows, :])
        P.dma("sp", un[:], pn[rows, :])
        P.tt("pool", up[:], up[:], u[:], ALU.subtract)
        P.tt("pool", up[:], up[:], mub[0][:], ALU.mult)
        P.tt("dve", un[:], un[:], u[:], ALU.subtract)
        P.tt("dve", un[:], un[:], mub[1][:], ALU.mult)
        P.tt("pool", u[:], u[:], up[:], ALU.add)
        P.tt("dve", u[:], u[:], un[:], ALU.add)
        r_, k_, v_ = u[:, 0:512], u[:, 512:1024], u[:, 1024:1536]
        P.copy("pool", q[:, 0:512], r_)
        P.copy("pool", q[:, 512:1024], v_)
        P.actf(X[:, 0:64], u[:, 1536:1600], AF.Tanh)
        P.copy("pool", X[:, 64:128], u[:, 1600:1664])
        P.actf(X[:, 128:224], u[:, 1664:1760], AF.Sigmoid)
        P.tr(tpA[:, 0:128], X[:, 0:128], identf[:])
        P.tr(tpA[:, 128:256], X[:, 128:256], identf[:])
        P.copy("act", XT[:], tpA[:])
        for i in range(4):
            P.mm(lin[i][:], XT[:, 0:128], lw[:, i * 512:(i + 1) * 512])
        P.mm(lin[4][:], XT[:, 128:256], lg[:])
        P.copy("act", q[:, 9 * 512:10 * 512], lin[4][:])
        P.tt("pool", kr[:], k_, vb[0][:], ALU.mult)
        P.tt("pool", sq[:], kr[:], kr[:], ALU.mult)
        P.op("dve", lambda e: e.reduce_sum(out=s8[:], in_=sq[:].rearrange("p (h j) -> p h j", h=8), axis=AX.X), [sq], [s8])
        P.actf(s8[:], s8[:], AF.Sqrt)
        P.ts("dve", s8[:], s8[:], 1e-12, ALU.max)
        P.op("dve", lambda e: e.reciprocal(out=s8[:], in_=s8[:]), [s8], [s8])
        kk3 = q[:, 2 * 512:3 * 512].rearrange("p (h j) -> p h j", h=8)
        P.tt("dve", kk3, kr[:].rearrange("p (h j) -> p h j", h=8), s8[:].unsqueeze(2).broadcast_to([128, 8, 64]), ALU.mult)
        for d in range(2):
            P.tt("dve", tw[d][:], lin[d][:], w0b[d][:], ALU.add)
            P.actf(tw[d][:], tw[d][:], AF.Sigmoid)
            P.actf(q[:, (3 + d) * 512:(4 + d) * 512], tw[d][:], AF.Exp, scale=-0.6065306597126334)
            P.tt("dve", ad[d][:], lin[2 + d][:], a0b[d][:], ALU.add)
            P.actf(ad[d][:], ad[d][:], AF.Sigmoid)
            P.stt("dve", t5[:], ad[d][:], -1.0, vb[1][:], ALU.add, ALU.mult)
            P.stt("dve", q[:, (5 + d) * 512:(6 + d) * 512], t5[:], 1.0, k_, ALU.add, ALU.mult)
            P.tt("pool", q[:, (7 + d) * 512:(8 + d) * 512], q[:, 2 * 512:3 * 512], ad[d][:], ALU.mult)
        P.tt("pool", t5[:], q[:, 5 * 512:6 * 512], q[:, 6 * 512:7 * 512], ALU.add)
        P.tt("pool", t5[:], t5[:], r_, ALU.mult)
        P.tt("pool", t5[:], t5[:], vb[2][:], ALU.mult)
        P.op("dve", lambda e, q=q: e.reduce_sum(out=q[:, 5120:5128], in_=t5[:].rearrange("p (h j) -> p h j", h=8), axis=AX.X), [t5], [q])
        P.dma("pool", qo[rows, :], q[:])
    return P


def run_rwprep(p, mu, w0, w2, a0, a2, g2, k_k, k_a, r_k):
    pcen = np.ascontiguousarray(p[:, :RWC])
    pprev, pnext = seg_shift(pcen, 1), seg_shift(pcen, -1)
    vec = np.stack([k_k, k_a, r_k.reshape(-1)], 0)
    LW = np.zeros((128, 2048), np.float32)
    for i, m_ in enumerate((w2[0], w2[1], a2[0], a2[1])):
        LW[i * 32:(i + 1) * 32, i * 512:(i + 1) * 512] = m_
    LG = np.zeros((128, 512), np.float32)
    LG[:96] = g2
    maps = []
    for core in range(NCORES):
        r = slice(core * TPC, (core + 1) * TPC)
        maps.append({"pc": pcen[r], "pp": pprev[r], "pn": pnext[r], "mu": mu, "w0": w0, "a0": a0, "vec": vec,
                     "lw": LW, "lg": LG, "identf": IDENTF})
    res = launch("rwprep", build_rwprep, maps)
    return np.concatenate([res[c]["q"] for c in range(NCORES)], 0)


NSTEP = SEQ + CTXL
NS = 8


def build_rwscan():
    P = Prog()
    rows = P.dram("rows", [2, NSTEP, 1280])
    vd = P.dram("vd", [128, NSTEP * 4])
    E_d = P.dram("E", [2, 128])
    yo = P.dram("y", [128, NSTEP * 4], kind="ExternalOutput")
    E = P.sb("Es", [2, 128])
    P.dma("sp", E[:], E_d)
    S = P.sb("S", [128, 256])
    P.op("dve", lambda e: e.memset(S[:], 0.0), [], [S])
    rb = [P.sb("rb%d" % i, [2, NS, 1280]) for i in range(2)]
    vb = [P.sb("vb%d" % i, [128, NS * 4]) for i in range(2)]
    yb = [P.sb("yb%d" % i, [128, NS * 4]) for i in range(2)]
    bc = [[P.ps("bc%d_%d" % (i, j), [128, 512]) for j in range(3)] for i in range(2)]
    t1 = P.sb("t1", [128, 256])
    t2 = P.sb("t2", [128, 256])
    t3 = P.sb("t3", [128, 256])
    t4 = P.sb("t4", [128, 256])
    sa = P.sb("sa", [128, 4])
    v3 = lambda ap: ap.rearrange("p (b j) -> p b j", b=4)
    for ch in range(NSTEP // NS):
        b2 = ch % 2
        P.dma("sp", rb[b2][:], rows[:, ch * NS:(ch + 1) * NS, :])
        P.dma("act", vb[b2][:], vd[:, ch * NS * 4:(ch + 1) * NS * 4])
        for s in range(NS):
            n = ch * NS + s
            pbk = bc[n % 2]
            P.mm(pbk[0][:], E[:], rb[b2][:, s, 0:512])
            P.mm(pbk[1][:], E[:], rb[b2][:, s, 512:1024])
            P.mm(pbk[2][:, 0:256], E[:], rb[b2][:, s, 1024:1280])
            KK, W, BE, KD, R = pbk[0][:, 0:256], pbk[0][:, 256:512], pbk[1][:, 0:256], pbk[1][:, 256:512], pbk[2][:, 0:256]
            vcol = vb[b2][:, s * 4:(s + 1) * 4]
            P.tt("dve", t1[:], S[:], KK, ALU.mult)
            P.op("dve", lambda e: e.reduce_sum(out=sa[:], in_=v3(t1[:]), axis=AX.X), [t1], [sa])
            P.tt("dve", S[:], S[:], W, ALU.mult)
            P.tt("dve", v3(t2[:]), v3(BE), sa[:].unsqueeze(2).broadcast_to([128, 4, 64]), ALU.mult)
            P.tt("dve", S[:], S[:], t2[:], ALU.subtract)
            P.tt("dve", v3(t3[:]), v3(KD), vcol.unsqueeze(2).broadcast_to([128, 4, 64]), ALU.mult)
            P.tt("dve", S[:], S[:], t3[:], ALU.add)
            P.tt("dve", t4[:], S[:], R, ALU.mult)
            P.op("dve", lambda e, yt=yb[b2], s=s: e.reduce_sum(out=yt[:, s * 4:(s + 1) * 4], in_=v3(t4[:]), axis=AX.X), [t4], [yb[b2]])
        P.dma("pool", yo[:, ch * NS * 4:(ch + 1) * NS * 4], yb[b2][:])
    return P


def scan_order():
    idx = np.zeros((2, NSTEP, B), np.int64)
    for b in range(B):
        cf = B * SEQ + b * CTXL + np.arange(CTXL)
        lf = b * SEQ + np.arange(SEQ)
        idx[0, :, b] = np.concatenate([cf, lf])
        idx[1, :, b] = np.concatenate([cf[::-1], lf[::-1]])
    return idx


def run_rwscan(q):
    idx = scan_order()
    col = lambda k, h: slice(k * 512 + h * 64, k * 512 + (h + 1) * 64)
    Emat = np.zeros((2, 128), np.float32)
    Emat[0, :64] = 1.0
    Emat[1, 64:] = 1.0
    maps = []
    for h in range(NCORES):
        rows = np.zeros((2, NSTEP, 5, B, 64), np.float32)
        vd = np.zeros((2, 64, NSTEP, B), np.float32)
        for d in range(2):
            ii = idx[d]
            for oi, k in enumerate((2, 3 + d, 7 + d, 5 + d, 0)):
                rows[d, :, oi] = q[ii][:, :, col(k, h)]
            vd[d] = q[ii][:, :, col(1, h)].transpose(2, 0, 1)
        maps.append({"rows": rows.reshape(2, NSTEP, 1280), "vd": vd.reshape(128, NSTEP * 4), "E": Emat})
    res = launch("rwscan", build_rwscan, maps)
    ys = [np.zeros((NTOK, 512), np.float32) for _ in range(2)]
    for h in range(NCORES):
        y = res[h]["y"].reshape(2, 64, NSTEP, B)
        for d in range(2):
            for b in range(B):
                ys[d][idx[d][:, b], h * 64:(h + 1) * 64] = y[d, :, :, b].T
    return ys


def build_rwout():
    P = Prog()
    yf = P.dram("yf", [TPC, 512])
    ybk = P.dram("yb", [TPC, 512])
    q = P.dram("q", [TPC, QC])
    gn = P.dram("gn", [2, 512])
    out = P.dram("rw", [TPC, 512], kind="ExternalOutput")
    gnb = [P.sb("gnb%d" % i, [128, 512]) for i in range(2)]
    for i in range(2):
        P.dma("sp", gnb[i][:], gn[i:i + 1, :].partition_broadcast(128))
    a = [P.sb("a%d" % i, [128, 512]) for i in range(2)]
    bq = [P.sb("bq%d" % i, [128, 512]) for i in range(2)]
    vv = [P.sb("vv%d" % i, [128, 512]) for i in range(2)]
    gg = [P.sb("gg%d" % i, [128, 512]) for i in range(2)]
    bo = [P.sb("bo%d" % i, [128, 8]) for i in range(2)]
    m8 = P.sb("m8", [128, 8])
    sq = P.sb("sq", [128, 512])
    ot = [P.sb("ot%d" % i, [128, 512]) for i in range(2)]
    h3 = lambda ap: ap.rearrange("p (h j) -> p h j", h=8)
    b3 = lambda ap: ap.unsqueeze(2).broadcast_to([128, 8, 64])
    for t in range(NT):
        b2 = t % 2
        rows = slice(t * 128, (t + 1) * 128)
        P.dma("sp", a[b2][:], yf[rows, :])
        P.dma("act", bq[b2][:], ybk[rows, :])
        P.dma("sp", vv[b2][:], q[rows, 512:1024])
        P.dma("act", gg[b2][:], q[rows, 4608:5120])
        P.dma("sp", bo[b2][:], q[rows, 5120:5128])
        y = a[b2]
        P.tt("dve", y[:], y[:], bq[b2][:], ALU.add)
        P.op("dve", lambda e, y=y: e.reduce_sum(out=m8[:], in_=h3(y[:]), axis=AX.X), [y], [m8])
        P.ts("dve", m8[:], m8[:], -1.0 / 64, ALU.mult)
        P.tt("dve", h3(y[:]), h3(y[:]), b3(m8[:]), ALU.add)
        P.tt("pool", sq[:], y[:], y[:], ALU.mult)
        P.op("dve", lambda e: e.reduce_sum(out=m8[:], in_=h3(sq[:]), axis=AX.X), [sq], [m8])
        P.ts("dve", m8[:], m8[:], 1.0 / 64, ALU.mult, 64e-5, ALU.add)
        P.actf(m8[:], m8[:], AF.Sqrt)
        P.op("dve", lambda e: e.reciprocal(out=m8[:], in_=m8[:]), [m8], [m8])
        P.tt("dve", h3(y[:]), h3(y[:]), b3(m8[:]), ALU.mult)
        P.tt("pool", y[:], y[:], gnb[0][:], ALU.mult)
        P.tt("pool", y[:], y[:], gnb[1][:], ALU.add)
        P.tt("dve", h3(vv[b2][:]), h3(vv[b2][:]), b3(bo[b2][:]), ALU.mult)
        P.tt("dve", y[:], y[:], vv[b2][:], ALU.add)
        P.tt("pool", ot[b2][:], y[:], gg[b2][:], ALU.mult)
        P.dma("pool", out[rows, :], ot[b2][:])
    return P


def run_rwout(yf, yb, q, gn):
    maps = []
    for core in range(NCORES):
        r = slice(core * TPC, (core + 1) * TPC)
        maps.append({"yf": yf[r], "yb": yb[r], "q": q[r], "gn": gn})
    res = launch("rwout", build_rwout, maps)
    return np.concatenate([res[c]["rw"] for c in range(NCORES)], 0)


TWO_PI = 2.0 * math.pi


def build_hyena(L):
    P = Prog()
    KT = L // 128
    FT = KT + 1
    Fp = FT * 128
    NB = B * 64
    zT = P.dram("zT", [128, L])
    win = P.dram("win", [L, 64])
    tabC = P.dram("tabC", [L, Fp], BF16)
    tabS = P.dram("tabS", [L, Fp], BF16)
    tabCi = P.dram("tabCi", [Fp, L], BF16)
    tabSi = P.dram("tabSi", [Fp, L], BF16)
    cfn_d = P.dram("cfn", [128, FT])
    U3 = P.dram("U3", [3, L, 3 * NB])
    cw_d = P.dram("cw", [4, 3 * NB])
    fw = P.dram("fw", [128, 64 + 64 + 256])
    fc = P.dram("fc", [128, 4])
    hb_d = P.dram("hb", [2, NB])
    out = P.dram("hy", [L, NB], kind="ExternalOutput")

    cfn = P.sb("cfns", [128, FT])
    P.dma("sp", cfn[:], cfn_d)
    cwb = P.sb("cwb", [128, 4, 3 * NB])
    for i in range(4):
        P.dma("act", cwb[:, i, :], cw_d[i:i + 1, :].partition_broadcast(128))
    fws = P.sb("fws", [128, 384])
    P.dma("sp", fws[:], fw)
    fcs = P.sb("fcs", [128, 4])
    P.dma("sp", fcs[:], fc)
    fk = P.sb("fk", [128, 2])
    for li in range(2):
        P.ts("dve", fk[:, li:li + 1], fcs[:, 2 * li + 1:2 * li + 2], fcs[:, 2 * li:2 * li + 1], ALU.mult, 0.0, ALU.add)
    hbb = P.sb("hbb", [128, 2, NB])
    for i in range(2):
        P.dma("act", hbb[:, i, :], hb_d[i:i + 1, :].partition_broadcast(128))
    ones = P.sb("ones", [128, 128])
    P.op("dve", lambda e: e.memset(ones[:], 1.0), [], [ones])
    pb = [P.ps("pb%d" % i, [128, 512]) for i in range(8)]

    hs = P.sb("hs", [128, KT, 128], BF16)
    hd = P.sb("hd", [128, KT, 128], BF16)
    hh = P.sb("hh", [128, 1, 256])
    PC = min(512, L)
    zt = P.sb("zt", [128, PC])
    rr = P.sb("rr", [128, PC])
    ri = P.sb("ri", [128, PC], mybir.dt.int32)
    h1 = P.sb("h1", [128, PC])
    h2 = P.sb("h2", [128, PC])
    P.op("dve", lambda e: e.memset(h1[:], 0.0), [], [h1])
    P.op("dve", lambda e: e.memset(h2[:], 0.0), [], [h2])
    wn = P.sb("wn", [128, 64])
    ab = P.sb("ab", [128, 256])
    for pc0 in range(0, L, PC):
        P.dma("sp", zt[:], zT[:, pc0:pc0 + PC])
        for li, (hin, hout, wsl) in enumerate(((zt, h1, fws[:, 0:64]), (h1, h2, fws[:, 64:128]))):
            P.mm(pb[0][0:64, 0:PC], wsl, hin[:])
            P.ts("dve", hout[0:64, :], pb[0][0:64, 0:PC], fcs[0:64, 2 * li:2 * li + 1], ALU.mult, fk[0:64, li:li + 1], ALU.add)
            P.ts("dve", rr[0:64, :], hout[0:64, :], 1.0 / TWO_PI, ALU.mult)
            P.copy("dve", ri[0:64, :], rr[0:64, :])
            P.copy("dve", rr[0:64, :], ri[0:64, :])
            P.stt("dve", hout[0:64, :], rr[0:64, :], -TWO_PI, hout[0:64, :], ALU.mult, ALU.add)
            P.ts("dve", rr[0:64, :], hout[0:64, :], math.pi, ALU.is_gt)
            P.stt("dve", hout[0:64, :], rr[0:64, :], -TWO_PI, hout[0:64, :], ALU.mult, ALU.add)
            P.ts("dve", rr[0:64, :], hout[0:64, :], -math.pi, ALU.is_lt)
            P.stt("dve", hout[0:64, :], rr[0:64, :], TWO_PI, hout[0:64, :], ALU.mult, ALU.add)
            P.actf(hout[0:64, :], hout[0:64, :], AF.Sin)
        for j in range(PC // 128):
            kt = pc0 // 128 + j
            P.mm(pb[1][:, 0:256], h2[:, j * 128:(j + 1) * 128], fws[:, 128:384])
            P.dma("act", wn[:], win[kt * 128:(kt + 1) * 128, :])
            P.tt("dve", hh[:, 0, :].rearrange("p (a c) -> p a c", a=4), pb[1][:, 0:256].rearrange("p (a c) -> p a c", a=4),
                 wn[:].unsqueeze(1).broadcast_to([128, 4, 64]), ALU.mult)
            if kt == 0:
                hv = hh[0:1, 0, :].rearrange("p (o d c) -> p o d c", o=2, d=2)
                P.op("dve", lambda e, hv=hv: e.memset(hv[:, :, 1, :], 0.0), [hh], [hh])
            P.actf(ab[:], hh[:, 0, :], AF.Abs)
            P.mm(pb[2][:, 0:256], ones[:], ab[:], start=(kt == 0), stop=(kt == KT - 1))
            h4 = hh[:, 0, :].rearrange("p (o d c) -> p o d c", o=2, d=2)
            P.tt("dve", hs[:, kt, :].rearrange("p (o c) -> p o c", o=2), h4[:, :, 0, :], h4[:, :, 1, :], ALU.add)
            P.tt("pool", hd[:, kt, :].rearrange("p (o c) -> p o c", o=2), h4[:, :, 0, :], h4[:, :, 1, :], ALU.subtract)
    rn = P.sb("rn", [128, 128])
    n4 = pb[2][:, 0:256].rearrange("p (o d c) -> p o d c", o=2, d=2)
    nt = P.sb("nt", [128, 128])
    P.copy("act", nt[:].rearrange("p (o c) -> p o c", o=2), n4[:, :, 0, :])
    P.tt("dve", rn[:].rearrange("p (o c) -> p o c", o=2), nt[:].rearrange("p (o c) -> p o c", o=2), n4[:, :, 1, :], ALU.add)
    P.ts("dve", rn[:], rn[:], 1e-6, ALU.add)
    P.op("dve", lambda e: e.reciprocal(out=rn[:], in_=rn[:]), [rn], [rn])

    tb = [[P.sb("tb%d_%d" % (i, j), [128, 256], BF16) for j in range(2)] for i in range(4)]
    tbi = [0]

    def fwd_dft(rhsC, rhsS, N, epi):
        for g0 in range(0, FT, 2):
            nm = min(2, FT - g0)
            for k in range(KT):
                t = tb[tbi[0] % 4]
                tbi[0] += 1
                P.dma("sp", t[0][:, 0:nm * 128], tabC[k * 128:(k + 1) * 128, g0 * 128:(g0 + nm) * 128])
                P.dma("act", t[1][:, 0:nm * 128], tabS[k * 128:(k + 1) * 128, g0 * 128:(g0 + nm) * 128])
                for mi in range(nm):
                    P.mm(pb[mi * 2][:, 0:N], t[0][:, mi * 128:(mi + 1) * 128], rhsC[:, k, :], start=(k == 0), stop=(k == KT - 1))
                    P.mm(pb[mi * 2 + 1][:, 0:N], t[1][:, mi * 128:(mi + 1) * 128], rhsS[:, k, :], start=(k == 0), stop=(k == KT - 1))
            for mi in range(nm):
                epi(g0 + mi, pb[mi * 2][:, 0:N], pb[mi * 2 + 1][:, 0:N])

    def inv_dft(Yr, Ys, epi):
        for g0 in range(0, KT, 2):
            nm = min(2, KT - g0)
            for f in range(FT):
                t = tb[tbi[0] % 4]
                tbi[0] += 1
                P.dma("sp", t[0][:, 0:nm * 128], tabCi[f * 128:(f + 1) * 128, g0 * 128:(g0 + nm) * 128])
                P.dma("act", t[1][:, 0:nm * 128], tabSi[f * 128:(f + 1) * 128, g0 * 128:(g0 + nm) * 128])
                for mi in range(nm):
                    P.mm(pb[4 + mi][:, 0:NB], t[0][:, mi * 128:(mi + 1) * 128], Yr[:, f, :], start=(f == 0), stop=False)
                    P.mm(pb[4 + mi][:, 0:NB], t[1][:, mi * 128:(mi + 1) * 128], Ys[:, f, :], start=False, stop=(f == FT - 1))
            for mi in range(nm):
                epi(g0 + mi, pb[4 + mi][:, 0:NB])

    Kr = P.sb("Kr", [128, FT, 128])
    Ks = P.sb("Ks", [128, FT, 128])

    def epi_k(m, aC, aS):
        P.stt("dve", Kr[:, m, :], aC, cfn[:, m:m + 1], rn[:], ALU.mult, ALU.mult)
        P.stt("dve", Ks[:, m, :], aS, cfn[:, m:m + 1], rn[:], ALU.mult, ALU.mult)

    fwd_dft(hs, hd, 128, epi_k)

    V = P.sb("V", [128, KT, NB], BF16)
    X1 = P.sb("X1", [128, KT, NB], BF16)
    X2 = P.sb("X2", [128, KT, NB], BF16)
    Z = P.sb("Z", [128, KT, NB], BF16)
    us = [[P.sb("us%d_%d" % (i, j), [128, 3 * NB]) for j in range(3)] for i in range(2)]
    for kt in range(KT):
        b2 = kt % 2
        for sft in range(3):
            P.dma("sp" if sft != 1 else "act", us[b2][sft][:], U3[sft, kt * 128:(kt + 1) * 128, :])
        a0 = us[b2][1]
        P.tt("dve", a0[:], a0[:], cwb[:, 1, :], ALU.mult)
        P.tt("pool", us[b2][0][:], us[b2][0][:], cwb[:, 0, :], ALU.mult)
        P.tt("pool", us[b2][2][:], us[b2][2][:], cwb[:, 2, :], ALU.mult)
        P.tt("dve", a0[:], a0[:], us[b2][0][:], ALU.add)
        P.tt("dve", a0[:], a0[:], us[b2][2][:], ALU.add)
        P.tt("dve", a0[:], a0[:], cwb[:, 3, :], ALU.add)
        P.copy("act", V[:, kt, :], a0[:, 0:NB])
        P.copy("act", X1[:, kt, :], a0[:, NB:2 * NB])
        P.copy("pool", X2[:, kt, :], a0[:, 2 * NB:3 * NB])

    Yr = P.sb("Yr", [128, FT, NB], BF16)
    Ys = P.sb("Ys", [128, FT, NB], BF16)
    ta = P.sb("ta", [128, NB])
    tc_ = P.sb("tc", [128, NB])
    ur = P.sb("ur", [128, NB])
    usb = P.sb("usb", [128, NB])
    b4 = lambda ap: ap.rearrange("p (b c) -> p b c", b=B)

    def make_epi_y(o):
        kr = lambda m: Kr[:, m, o * 64:(o + 1) * 64].unsqueeze(1).broadcast_to([128, B, 64])
        ks = lambda m: Ks[:, m, o * 64:(o + 1) * 64].unsqueeze(1).broadcast_to([128, B, 64])

        def epi(m, aC, aS):
            P.copy("act", ur[:], aC)
            P.copy("act", usb[:], aS)
            P.tt("dve", b4(ta[:]), b4(ur[:]), kr(m), ALU.mult)
            P.tt("pool", b4(tc_[:]), b4(usb[:]), ks(m), ALU.mult)
            P.tt("dve", Yr[:, m, :], ta[:], tc_[:], ALU.subtract)
            P.tt("dve", b4(ta[:]), b4(ur[:]), ks(m), ALU.mult)
            P.tt("pool", b4(tc_[:]), b4(usb[:]), kr(m), ALU.mult)
            P.tt("dve", Ys[:, m, :], ta[:], tc_[:], ALU.add)
        return epi

    ot = [P.sb("ot%d" % i, [128, NB]) for i in range(2)]

    def epi_z(kt, acc):
        P.tt("dve", ta[:], V[:, kt, :], hbb[:, 0, :], ALU.mult)
        P.tt("dve", ta[:], ta[:], acc, ALU.add)
        P.tt("dve", Z[:, kt, :], ta[:], X1[:, kt, :], ALU.mult)

    def epi_o(kt, acc):
        o_ = ot[kt % 2]
        P.tt("dve", tc_[:], Z[:, kt, :], hbb[:, 1, :], ALU.mult)
        P.tt("dve", tc_[:], tc_[:], acc, ALU.add)
        P.tt("dve", o_[:], tc_[:], X2[:, kt, :], ALU.mult)
        P.dma("pool", out[kt * 128:(kt + 1) * 128, :], o_[:])

    fwd_dft(V, V, NB, make_epi_y(0))
    inv_dft(Yr, Ys, epi_z)
    fwd_dft(Z, Z, NB, make_epi_y(1))
    inv_dft(Yr, Ys, epi_o)
    return P


def negpi(P):
    if not hasattr(P, "_negpi"):
        t = P.sb("negpi", [128, 1])
        P.op("dve", lambda e: e.memset(t[:], -math.pi), [], [t])
        P._negpi = t
    return P._negpi[0:64, :]


_HY_CONST = {}


def hyena_consts(L):
    if L in _HY_CONST:
        return _HY_CONST[L]
    KT = L // 128
    Fp = (KT + 1) * 128
    f32 = np.float32
    t = np.linspace(0.0, 1.0, L, dtype=f32)[:, None]
    pos = np.arange(L, dtype=f32)[:, None]
    bands = np.linspace(1e-4, 15, 16, dtype=f32)[None, :]
    ang = (f32(2.0 * math.pi) * pos * bands / f32(L)).astype(f32)
    z = np.concatenate([t, np.cos(ang), -np.sin(ang)], -1).astype(f32)
    zT = np.zeros((128, L), f32)
    zT[:33] = z.T
    deltas = np.linspace(math.log(1e-2) / 1.5, math.log(1e-2) / 0.3, 512, dtype=f32)
    win = np.exp(-t * np.abs(deltas)[None, :]).astype(f32)
    a = np.arange(L, dtype=np.int64)[:, None]
    bq = np.arange(Fp, dtype=np.int64)[None, :]
    th = ((a * bq) % (2 * L)).astype(np.float64) * (math.pi / L)
    tabC = np.cos(th).astype(f32).astype(NPBF)
    tabS = np.sin(th).astype(f32).astype(NPBF)
    cf = np.zeros(Fp, f32)
    cf[:L + 1] = 2.0 / (2 * L)
    cf[0] = cf[L] = 1.0 / (2 * L)
    cfn = np.ascontiguousarray(cf.reshape(KT + 1, 128).T)
    _HY_CONST[L] = dict(zT=zT, win=win, tabC=tabC, tabS=tabS, tabCi=np.ascontiguousarray(tabC.T),
                        tabSi=np.ascontiguousarray(tabS.T), cfn=cfn)
    return _HY_CONST[L]


def run_hyena(pseg, L, conv_w, conv_b, f_w1, f_b1, f_w2, f_b2, f_w3, f_freq, hy_b):
    C = hyena_consts(L)
    prev = np.zeros_like(pseg)
    nxt = np.zeros_like(pseg)
    prev[:, 1:] = pseg[:, :-1]
    nxt[:, :-1] = pseg[:, 1:]
    maps = []
    for core in range(NCORES):
        ch = slice(core * 64, (core + 1) * 64)
        cols = np.concatenate([np.arange(k * 512 + core * 64, k * 512 + (core + 1) * 64) for k in range(3)])
        U3 = np.stack([a[:, :, cols].reshape(B, L, 3, 64).transpose(1, 2, 0, 3).reshape(L, 3 * B * 64) for a in (prev, pseg, nxt)], 0)
        cw = np.stack([np.broadcast_to(v_[cols].reshape(3, 1, 64), (3, B, 64)).reshape(-1) for v_ in (conv_w[0], conv_w[1], conv_w[2], conv_b)], 0)
        fw = np.zeros((128, 384), np.float32)
        fw[:33, 0:64] = f_w1
        fw[:64, 64:128] = f_w2
        fw[:64, 128:384] = f_w3.reshape(64, 2, 2, 512)[:, :, :, ch].reshape(64, 256)
        fcv = np.zeros((128, 4), np.float32)
        fcv[:64] = np.stack([f_freq[0], f_b1, f_freq[1], f_b2], 1)
        maps.append({"zT": C["zT"], "win": np.ascontiguousarray(C["win"][:, ch]), "tabC": C["tabC"], "tabS": C["tabS"],
                     "tabCi": C["tabCi"], "tabSi": C["tabSi"], "cfn": C["cfn"], "U3": np.ascontiguousarray(U3),
                     "cw": np.ascontiguousarray(cw), "fw": fw, "fc": fcv,
                     "hb": np.ascontiguousarray(np.broadcast_to(hy_b[:, None, ch], (2, B, 64)).reshape(2, B * 64))})
    res = launch(("hyena", L), lambda: build_hyena(L), maps)
    hy = np.zeros((B, L, 512), np.float32)
    for core in range(NCORES):
        hy[:, :, core * 64:(core + 1) * 64] = res[core]["hy"].reshape(L, B, 64).transpose(1, 0, 2)
    return hy


def kernel(x, c, ctx, c_ctx, ada_w, ada_b, norm_g, final_g, ev_w_in, ev_w_out,
           rw_mu, rw_w0, rw_w2, rw_a0, rw_a2, rw_g2, rw_kk, rw_ka, rw_rk, rw_gn,
           hy_conv_w, hy_conv_b, hy_f_w1, hy_f_b1, hy_f_w2, hy_f_b2, hy_f_w3, hy_freq, hy_bias,
           da_w_qkv, da_w_out, da_lambda, da_subln, moe_router, moe_w1, moe_w3, moe_w2):
    f = lambda a: np.ascontiguousarray(np.asarray(a, dtype=np.float32))
    (x, c, ctx, c_ctx, ada_w, ada_b, norm_g, final_g, ev_w_in, ev_w_out, rw_mu, rw_w0, rw_w2, rw_a0, rw_a2, rw_g2,
     rw_kk, rw_ka, rw_rk, rw_gn, hy_conv_w, hy_conv_b, hy_f_w1, hy_f_b1, hy_f_w2, hy_f_b2, hy_f_w3, hy_freq, hy_bias,
     da_w_qkv, da_w_out, da_lambda, da_subln, moe_router, moe_w1, moe_w3, moe_w2) = [f(a) for a in (
        x, c, ctx, c_ctx, ada_w, ada_b, norm_g, final_g, ev_w_in, ev_w_out, rw_mu, rw_w0, rw_w2, rw_a0, rw_a2, rw_g2,
        rw_kk, rw_ka, rw_rk, rw_gn, hy_conv_w, hy_conv_b, hy_f_w1, hy_f_b1, hy_f_w2, hy_f_b2, hy_f_w3, hy_freq, hy_bias,
        da_w_qkv, da_w_out, da_lambda, da_subln, moe_router, moe_w1, moe_w3, moe_w2)]
    mods = run_mod(c, c_ctx, ada_w, ada_b)
    xcur = np.ascontiguousarray(np.concatenate([x.reshape(-1, D), ctx.reshape(-1, D)], 0))
    prev = None
    NL = B * SEQ
    for i in range(4):
        j = i // 2
        if i % 2 == 0:
            xcur, p = run_tok_a(xcur, mods[i], norm_g[i, 0], ev_w_in[j], prev=prev)
            q = run_rwprep(p, rw_mu[j], rw_w0[j], rw_w2[j], rw_a0[j], rw_a2[j], rw_g2[j], rw_kk[j], rw_ka[j], rw_rk[j])
            yf, yb = run_rwscan(q)
            rw = run_rwout(yf, yb, q, rw_gn[j])
            hargs = (hy_conv_w[j], hy_conv_b[j], hy_f_w1[j], hy_f_b1[j], hy_f_w2[j], hy_f_b2[j], hy_f_w3[j], hy_freq[j], hy_bias[j])
            hyl = run_hyena(np.ascontiguousarray(p[:NL, RWC:].reshape(B, SEQ, 1536)), SEQ, *hargs)
            hyc = run_hyena(np.ascontiguousarray(p[NL:, RWC:].reshape(B, CTXL, 1536)), CTXL, *hargs)
            mix = np.ascontiguousarray(np.concatenate([rw, np.concatenate([hyl.reshape(-1, 512), hyc.reshape(-1, 512)], 0)], 1))
            w_out = ev_w_out[j]
        else:
            lam_init = 0.8 - 0.6 * math.exp(-0.3 * i)
            xcur, p = run_tok_a(xcur, mods[i], norm_g[i, 0], da_w_qkv[j], prev=prev, rope=True, pbf=True)
            mix = run_attn(p, da_lambda[j], da_subln[j], lam_init)
            if i < 3:
                mix[NL:] = run_attn_ctx(p, da_lambda[j], da_subln[j], lam_init)
            w_out = da_w_out[j]
        x1, h2, aff = run_tok_c(mix, xcur, mods[i], norm_g[i, 1], w_out, moe_router[i])
        G = run_topk(aff)
        y = run_moe(h2, G, moe_w1[i], moe_w3[i], moe_w2[i])
        xcur, prev = x1, (y, mods[i])
    zero_mod = np.zeros((5, 6 * D), np.float32)
    _, out = run_tok_a(xcur, zero_mod, final_g, None, prev=prev, final=True)
    return np.ascontiguousarray(out[:NL].reshape(B, SEQ, D).astype(np.float32))
```

```python
import math
import numpy as np
import ml_dtypes
import concourse.bass as bass
import concourse.mybir as mybir
from concourse.bass_utils import run_bass_kernel_spmd

F32 = mybir.dt.float32
BF16 = mybir.dt.bfloat16
AF = mybir.ActivationFunctionType
ALU = mybir.AluOpType
AX = mybir.AxisListType
NPBF = ml_dtypes.bfloat16

ENGS = ("pe", "act", "dve", "pool", "sp")
N_DMA_SEMS = 6
RELAX_DVE = True
NCORES = 8


class Prog:
    def __init__(self):
        self.nc = bass.Bass("TRN2", target_bir_lowering=False)
        self.ops = {e: [] for e in ENGS}
        self.cnt = {e: 0 for e in ENGS}
        self.dma_cnt = {e: [0] * N_DMA_SEMS for e in ENGS}
        self.dma_rr = {e: 0 for e in ENGS}
        self.last_w = {}
        self.readers = {}
        self.seen = {e: {} for e in ENGS}
        self._ctx = []
        self.n_inst = 0
        self._uid = 0

    def sb(self, name, shape, dt=F32):
        g = self.nc.sbuf_tensor(name, list(shape), dt)
        t = g.__enter__()
        self._ctx.append(g)
        return t

    def ps(self, name, shape, dt=F32):
        g = self.nc.psum_tensor(name, list(shape), dt)
        t = g.__enter__()
        self._ctx.append(g)
        return t

    def dram(self, name, shape, dt=F32, kind="ExternalInput"):
        if kind is None:
            return self.nc.dram_tensor(name, list(shape), dt).ap()
        return self.nc.dram_tensor(name, list(shape), dt, kind=kind).ap()

    @staticmethod
    def _key(x):
        if isinstance(x, (str, tuple)):
            return x
        t = getattr(x, "tensor", x)
        return getattr(t, "name", None) or str(t)

    def _deps(self, reads, writes):
        toks = []
        for r in reads:
            k = self._key(r)
            if k in self.last_w:
                toks.append(self.last_w[k])
        for w in writes:
            k = self._key(w)
            if k in self.last_w:
                toks.append(self.last_w[k])
            toks.extend(self.readers.get(k, ()))
        return toks

    def _commit(self, tok, reads, writes):
        for r in reads:
            self.readers.setdefault(self._key(r), []).append(tok)
        for w in writes:
            k = self._key(w)
            self.last_w[k] = tok
            self.readers[k] = []

    def _waits(self, eng, toks):
        need = {}
        for (sname, val) in toks:
            if val > need.get(sname, 0):
                need[sname] = val
        out = []
        seen = self.seen[eng]
        for sname, val in need.items():
            if seen.get(sname, 0) >= val:
                continue
            seen[sname] = val
            out.append((sname, val))
        return out

    def op(self, eng, fn, reads=(), writes=()):
        toks = self._deps(reads, writes)
        if eng == "pe":
            toks = [t for t in toks if t[0] != "c_pe"]
        elif eng == "dve" and RELAX_DVE:
            cur = self.cnt["dve"]
            toks = [t for t in toks if not (t[0] == "c_dve" and t[1] <= cur - 1)]
        waits = self._waits(eng, toks)
        self.cnt[eng] += 1
        tok = ("c_" + eng, self.cnt[eng])
        self.ops[eng].append((waits, fn, ("c_" + eng, 1)))
        self._commit(tok, reads, writes)
        self.n_inst += 1
        return tok

    def dma(self, eng, out, in_, reads=None, writes=None, **kw):
        reads = [in_] if reads is None else reads
        writes = [out] if writes is None else writes
        toks = self._deps(reads, writes)
        i = self.dma_rr[eng]
        self.dma_rr[eng] = (i + 1) % N_DMA_SEMS
        sname = "d_%s_%d" % (eng, i)
        prev = self.dma_cnt[eng][i]
        if prev > 0:
            toks.append((sname, 16 * prev))
        waits = self._waits(eng, toks)
        self.dma_cnt[eng][i] = prev + 1
        tok = (sname, 16 * (prev + 1))

        def fn(e, out=out, in_=in_, kw=kw):
            return e.dma_start(out=out, in_=in_, **kw)

        self.ops[eng].append((waits, fn, (sname, 16)))
        self._commit(tok, reads, writes)
        self.n_inst += 1
        return tok

    def build(self):
        nc = self.nc
        toks = []
        for e in ENGS:
            if self.cnt[e]:
                toks.append(("c_" + e, self.cnt[e]))
            for i in range(N_DMA_SEMS):
                if self.dma_cnt[e][i]:
                    toks.append(("d_%s_%d" % (e, i), 16 * self.dma_cnt[e][i]))
        self.ops["sp"].append((self._waits("sp", toks), None, None))
        names = set()
        for e in ENGS:
            for waits, fn, inc in self.ops[e]:
                for s, _ in waits:
                    names.add(s)
                if inc:
                    names.add(inc[0])
        sems = {}
        for s in sorted(names):
            g = nc.semaphore(s)
            sems[s] = g.__enter__()
            self._ctx.append(g)
        blk = nc.Block()
        block = blk.__enter__()
        emap = {"pe": block.tensor, "act": block.scalar, "dve": block.vector,
                "pool": block.gpsimd, "sp": block.sync}
        for e in ENGS:
            lst = self.ops[e]
            if not lst:
                continue

            def body(eng, lst=lst):
                for waits, fn, inc in lst:
                    for s, v in waits:
                        eng.wait_ge(sems[s], v)
                    if fn is not None:
                        fn(eng).then_inc(sems[inc[0]], inc[1])

            emap[e](body)
        blk.__exit__(None, None, None)
        for g in reversed(self._ctx):
            g.__exit__(None, None, None)
        return nc

    def copy(self, eng, out, in_):
        if eng == "act":
            return self.op("act", lambda e: e.copy(out=out, in_=in_), [in_], [out])
        return self.op(eng, lambda e: e.tensor_copy(out=out, in_=in_), [in_], [out])

    def tt(self, eng, out, a, b, op):
        return self.op(eng, lambda e: e.tensor_tensor(out=out, in0=a, in1=b, op=op), [a, b], [out])

    def ts(self, eng, out, a, s1, op0, s2=None, op1=None, accum=None):
        rd = [a] + [s for s in (s1, s2) if not isinstance(s, (int, float, type(None)))]
        wr = [out] + ([accum] if accum is not None else [])
        kw = {}
        if op1 is not None:
            kw["op1"] = op1
        if accum is not None:
            kw["accum_out"] = accum
        return self.op(eng, lambda e: e.tensor_scalar(out=out, in0=a, scalar1=s1, scalar2=s2, op0=op0, **kw), rd, wr)

    def stt(self, eng, out, a, s, b, op0, op1):
        rd = [a, b] + ([s] if not isinstance(s, (int, float)) else [])
        return self.op(eng, lambda e: e.scalar_tensor_tensor(out=out, in0=a, scalar=s, in1=b, op0=op0, op1=op1), rd, [out])

    def actf(self, out, in_, func, bias=None, scale=None, accum=None):
        rd = [in_] + [s for s in (bias, scale) if s is not None and not isinstance(s, (int, float))]
        wr = [out] + ([accum] if accum is not None else [])
        kw = {}
        if bias is not None:
            kw["bias"] = bias
        if scale is not None:
            kw["scale"] = scale
        if accum is not None:
            kw["accum_out"] = accum
        return self.op("act", lambda e: e.activation(out=out, in_=in_, func=func, **kw), rd, wr)

    def mm(self, out, lhsT, rhs, start=True, stop=True):
        return self.op("pe", lambda e: e.matmul(out, lhsT, rhs, start=start, stop=stop), [lhsT, rhs], [out])

    def tr(self, out, in_, ident):
        return self.op("pe", lambda e: e.transpose(out, in_, ident), [in_, ident], [out])


_PROG_CACHE = {}
N_LAUNCH = [0]
TRACE = [False]


def launch(key, builder, in_maps):
    if key not in _PROG_CACHE:
        P = builder()
        P.build()
        _PROG_CACHE[key] = P
    P = _PROG_CACHE[key]
    N_LAUNCH[0] += 1
    if TRACE[0]:
        res = run_bass_kernel_spmd(P.nc, in_maps, core_ids=list(range(NCORES)), trace=True)
        print("EXEC_NS", key, res.exec_time_ns, flush=True)
    else:
        res = run_bass_kernel_spmd(P.nc, in_maps, core_ids=list(range(NCORES)))
    return res.results


D = 1024
B = 4
SEQ = 4096
CTXL = 256
NTOK = B * SEQ + B * CTXL
TPC = NTOK // NCORES
NT = TPC // 128
EPS = 1e-6


def tile_modrow(gt):
    return gt // 32 if gt < 128 else 4


def build_mod():
    P = Prog()
    cT = P.dram("cT", [D, 5])
    w = P.dram("w", [D, 3072])
    bias = P.dram("bias", [1, 3072])
    out = P.dram("out", [5, 3072], kind="ExternalOutput")
    ct = P.sb("ct", [128, 8, 5])
    sg = P.sb("sg", [128, 8, 5])
    st = P.sb("st", [128, 8, 5])
    bt = P.sb("bt", [5, 3072])
    ot = P.sb("ot", [5, 3072])
    wts = [P.sb("wt%d" % i, [128, 3072]) for i in range(8)]
    accs = [P.ps("acc%d" % i, [128, 512]) for i in range(6)]
    P.dma("sp", ct[:], cT.rearrange("(k p) r -> p k r", p=128))
    P.dma("act", bt[:], bias.partition_broadcast(5))
    for k in range(8):
        P.dma("sp" if k % 2 == 0 else "act", wts[k][:], w[k * 128:(k + 1) * 128, :])
    P.actf(sg[:], ct[:], AF.Sigmoid)
    P.tt("dve", st[:], ct[:], sg[:], ALU.mult)
    for n in range(6):
        for k in range(8):
            P.mm(accs[n][0:5, :], st[:, k, :], wts[k][:, n * 512:(n + 1) * 512], start=(k == 0), stop=(k == 7))
        P.tt("dve", ot[:, n * 512:(n + 1) * 512], accs[n][0:5, :], bt[:, n * 512:(n + 1) * 512], ALU.add)
    P.dma("sp", out, ot[:])
    return P


def run_mod(c, c_ctx, ada_w, ada_b):
    cT = np.ascontiguousarray(np.concatenate([c, c_ctx[None]], 0).T)
    maps = []
    for core in range(NCORES):
        i, hf = core // 2, core % 2
        maps.append({"cT": cT, "w": np.ascontiguousarray(ada_w[i][:, hf * 3072:(hf + 1) * 3072]),
                     "bias": np.ascontiguousarray(ada_b[i][None, hf * 3072:(hf + 1) * 3072])})
    res = launch("mod", build_mod, maps)
    mods = np.zeros((4, 5, 6144), np.float32)
    for core in range(NCORES):
        i, hf = core // 2, core % 2
        mods[i][:, hf * 3072:(hf + 1) * 3072] = res[core]["out"]
    return mods


def build_tok_a(ncols, combine, rope, final=False, pbf=False):
    P = Prog()
    x1 = P.dram("x1", [TPC, D])
    if combine:
        ya = P.dram("ya", [TPC, D])
        gm = P.dram("gm", [NT, D])
        xo = P.dram("xo", [TPC, D], kind="ExternalOutput")
    msh = P.dram("msh", [NT, D])
    msc = P.dram("msc", [NT, D])
    g = P.dram("g", [1, D])
    w = None if final else P.dram("w", [D, ncols])
    ident_d = P.dram("ident", [128, 128], BF16)
    if rope:
        rc = P.dram("rc", [TPC, 64])
        rs = P.dram("rs", [TPC, 64])
    pout = P.dram("p", [TPC, ncols], BF16 if pbf else F32, kind="ExternalOutput")

    ident = P.sb("identb", [128, 128], BF16)
    P.dma("sp", ident[:], ident_d)
    gbc = P.sb("gbc", [128, D])
    P.dma("sp", gbc[:], g.partition_broadcast(128))
    wb = P.sb("wb", [128, 8, ncols], BF16)
    wst = [P.sb("wst%d" % i, [128, ncols]) for i in range(2)]
    for k in range(0 if final else 8):
        P.dma("sp" if k % 2 == 0 else "act", wst[k % 2][:], w[k * 128:(k + 1) * 128, :])
        P.copy("pool" if k % 2 == 0 else "dve", wb[:, k, :], wst[k % 2][:])

    xts = [P.sb("xt%d" % i, [128, D]) for i in range(2)]
    if combine:
        yas = [P.sb("ya%d" % i, [128, D]) for i in range(2)]
        gms = [P.sb("gm%d" % i, [128, D]) for i in range(2)]
    scs = [P.sb("sc%d" % i, [128, D]) for i in range(2)]
    shs = [P.sb("sh%d" % i, [128, D]) for i in range(2)]
    junk = P.sb("junk", [128, D])
    ssq = [P.sb("ssq%d" % i, [128, 1]) for i in range(2)]
    rstd = [P.sb("rstd%d" % i, [128, 1]) for i in range(2)]
    h32 = P.sb("h32", [128, D])
    hb = [P.sb("hb%d" % i, [128, D], BF16) for i in range(2)]
    hT = [P.sb("hT%d" % i, [128, D], BF16) for i in range(2)]
    tp = [P.ps("tp%d" % i, [128, D], BF16) for i in range(2)]
    accs = [P.ps("acc%d" % i, [128, 512]) for i in range(4)]
    pts = [P.sb("pt%d" % i, [128, ncols]) for i in range(2)]
    if pbf:
        ptb = [P.sb("ptb%d" % i, [128, ncols], BF16) for i in range(2)]
    if rope:
        rcs = [P.sb("rc%d" % i, [128, 64]) for i in range(2)]
        rss = [P.sb("rs%d" % i, [128, 64]) for i in range(2)]
        t1 = P.sb("t1", [128, 2048])
        t2 = P.sb("t2", [128, 2048])
    nchunks = [(n0, min(512, ncols - n0)) for n0 in range(0, ncols, 512)]
    ai = 0
    for t in range(NT):
        b2 = t % 2
        rows = slice(t * 128, (t + 1) * 128)
        xt = xts[b2]
        P.dma("sp", xt[:], x1[rows, :])
        P.dma("sp", scs[b2][:], msc[t:t + 1, :].partition_broadcast(128))
        P.dma("sp", shs[b2][:], msh[t:t + 1, :].partition_broadcast(128))
        if combine:
            P.dma("act", yas[b2][:], ya[rows, :])
            P.dma("act", gms[b2][:], gm[t:t + 1, :].partition_broadcast(128))
            P.tt("pool", yas[b2][:], yas[b2][:], gms[b2][:], ALU.mult)
            P.tt("dve", xt[:], xt[:], yas[b2][:], ALU.add)
            P.dma("pool", xo[rows, :], xt[:])
        if rope:
            P.dma("act", rcs[b2][:], rc[rows, :])
            P.dma("act", rss[b2][:], rs[rows, :])
        P.actf(junk[:], xt[:], AF.Square, accum=ssq[b2][:])
        P.ts("dve", rstd[b2][:], ssq[b2][:], 1.0 / D, ALU.mult, EPS, ALU.add)
        P.actf(rstd[b2][:], rstd[b2][:], AF.Sqrt)
        P.op("dve", lambda e, o=rstd[b2]: e.reciprocal(out=o[:], in_=o[:]), [rstd[b2]], [rstd[b2]])
        P.ts("pool", scs[b2][:], scs[b2][:], 1.0, ALU.add)
        P.tt("pool", scs[b2][:], scs[b2][:], gbc[:], ALU.mult)
        P.stt("dve", h32[:], xt[:], rstd[b2][:], scs[b2][:], ALU.mult, ALU.mult)
        if final:
            P.tt("pool", h32[:], h32[:], shs[b2][:], ALU.add)
            P.dma("pool", pout[rows, :], h32[:])
            continue
        P.tt("pool", hb[b2][:], h32[:], shs[b2][:], ALU.add)
        for k in range(8):
            P.tr(tp[b2][:, k * 128:(k + 1) * 128], hb[b2][:, k * 128:(k + 1) * 128], ident[:])
        P.copy("act", hT[b2][:], tp[b2][:])
        pt = pts[b2]
        for ci, (n0, nw) in enumerate(nchunks):
            acc = accs[ai % 4]
            ai += 1
            for k in range(8):
                P.mm(acc[:, 0:nw], hT[b2][:, k * 128:(k + 1) * 128], wb[:, k, n0:n0 + nw], start=(k == 0), stop=(k == 7))
            P.copy("act" if ci % 2 == 0 else "dve", pt[:, n0:n0 + nw], acc[:, 0:nw])
        if rope:
            qk = pt[:, 0:2048]
            c64 = rcs[b2][:].unsqueeze(1).broadcast_to([128, 32, 64])
            P.tt("dve", t1[:].rearrange("p (n d) -> p n d", d=64), qk.rearrange("p (n d) -> p n d", d=64), c64, ALU.mult)
            xv = qk.rearrange("p (n a h f) -> p n a h f", a=2, h=2, f=16)
            tv = t2[:].rearrange("p (n a h f) -> p n a h f", a=2, h=2, f=16)
            sv = rss[b2][:].rearrange("p (a h f) -> p a h f", a=2, h=2)
            for hh in range(2):
                s_b = sv[:, :, hh, :].unsqueeze(1).broadcast_to([128, 32, 2, 16])
                P.tt("pool", tv[:, :, :, hh, :], xv[:, :, :, 1 - hh, :], s_b, ALU.mult)
            P.tt("dve", qk, t1[:], t2[:], ALU.add)
        if pbf:
            P.copy("act", ptb[b2][:], pt[:])
            P.dma("pool", pout[rows, :], ptb[b2][:])
        else:
            P.dma("pool", pout[rows, :], pt[:])
    return P


def rope_tables():
    L = SEQ
    row = np.repeat(np.arange(L // 64, dtype=np.float32), 64)
    col = np.tile(np.arange(64, dtype=np.float32), L // 64)
    inv = (10000.0 ** (-np.arange(16, dtype=np.float32) / 16)).astype(np.float32)
    ar = row[:, None] * inv
    ac = col[:, None] * inv
    C = np.concatenate([np.cos(ar), np.cos(ar), np.cos(ac), np.cos(ac)], 1).astype(np.float32)
    S = np.concatenate([-np.sin(ar), np.sin(ar), -np.sin(ac), np.sin(ac)], 1).astype(np.float32)
    Cf = np.concatenate([np.tile(C, (B, 1)), np.ones((B * CTXL, 64), np.float32)], 0)
    Sf = np.concatenate([np.tile(S, (B, 1)), np.zeros((B * CTXL, 64), np.float32)], 0)
    return Cf, Sf


def modrows(mods_i, k):
    out = []
    for core in range(NCORES):
        rows = [tile_modrow(core * NT + t) for t in range(NT)]
        out.append(np.ascontiguousarray(mods_i[rows, k * D:(k + 1) * D]))
    return out


IDENT = np.eye(128, dtype=np.float32).astype(NPBF)


def run_tok_a(x1, mods_i, g, w, prev=None, rope=False, final=False, pbf=False):
    ncols = D if final else w.shape[1]
    combine = prev is not None
    msh, msc = modrows(mods_i, 0), modrows(mods_i, 1)
    if combine:
        gm = modrows(prev[1], 5)
    if rope:
        Cf, Sf = rope_tables()
    maps = []
    for core in range(NCORES):
        r = slice(core * TPC, (core + 1) * TPC)
        m = {"x1": x1[r], "msh": msh[core], "msc": msc[core], "g": g[None, :], "ident": IDENT}
        if not final:
            m["w"] = w
        if combine:
            m.update(ya=prev[0][r], gm=gm[core])
        if rope:
            m.update(rc=Cf[r], rs=Sf[r])
        maps.append(m)
    res = launch(("tok_a", ncols, combine, rope, final, pbf), lambda: build_tok_a(ncols, combine, rope, final, pbf), maps)
    p = np.concatenate([res[c]["p"] for c in range(NCORES)], 0)
    xo = np.concatenate([res[c]["xo"] for c in range(NCORES)], 0) if combine else x1
    return xo, p


def build_tok_c():
    P = Prog()
    mix = P.dram("mix", [TPC, D])
    x = P.dram("x", [TPC, D])
    mg = P.dram("mg", [NT, D])
    msh = P.dram("msh", [NT, D])
    msc = P.dram("msc", [NT, D])
    g = P.dram("g", [1, D])
    w = P.dram("w", [D, D])
    rw = P.dram("rw", [D, 16])
    ident_d = P.dram("ident", [128, 128], BF16)
    identf_d = P.dram("identf", [128, 128])
    x1o = P.dram("x1o", [TPC, D], kind="ExternalOutput")
    h2o = P.dram("h2o", [TPC, D], BF16, kind="ExternalOutput")
    affo = P.dram("affo", [TPC, 16], kind="ExternalOutput")

    ident = P.sb("identb", [128, 128], BF16)
    identf = P.sb("identfs", [128, 128])
    P.dma("sp", ident[:], ident_d)
    P.dma("sp", identf[:], identf_d)
    gbc = P.sb("gbc", [128, D])
    P.dma("sp", gbc[:], g.partition_broadcast(128))
    rwt = P.sb("rwt", [128, 8, 16])
    P.dma("sp", rwt[:], rw.rearrange("(k p) e -> p k e", p=128))
    wb = P.sb("wb", [128, 8, D], BF16)
    wst = [P.sb("wst%d" % i, [128, D]) for i in range(2)]
    for k in range(8):
        P.dma("sp" if k % 2 == 0 else "act", wst[k % 2][:], w[k * 128:(k + 1) * 128, :])
        P.copy("pool" if k % 2 == 0 else "dve", wb[:, k, :], wst[k % 2][:])

    mts = [P.sb("mt%d" % i, [128, D]) for i in range(2)]
    xts = [P.sb("xt%d" % i, [128, D]) for i in range(2)]
    mgs = [P.sb("mg%d" % i, [128, D]) for i in range(2)]
    scs = [P.sb("sc%d" % i, [128, D]) for i in range(2)]
    shs = [P.sb("sh%d" % i, [128, D]) for i in range(2)]
    mb = [P.sb("mb%d" % i, [128, D], BF16) for i in range(2)]
    mT = [P.sb("mT%d" % i, [128, D], BF16) for i in range(2)]
    junk = P.sb("junk", [128, D])
    ssq = [P.sb("ssq%d" % i, [128, 1]) for i in range(2)]
    rstd = [P.sb("rstd%d" % i, [128, 1]) for i in range(2)]
    h32 = [P.sb("h32_%d" % i, [128, D]) for i in range(2)]
    hb = [P.sb("hb%d" % i, [128, D], BF16) for i in range(2)]
    hTf = [P.sb("hTf%d" % i, [128, D]) for i in range(2)]
    tp = [P.ps("tp%d" % i, [128, D], BF16) for i in range(2)]
    tpf = [P.ps("tpf%d" % i, [128, 512]) for i in range(2)]
    accs = [P.ps("acc%d" % i, [128, 512]) for i in range(2)]
    lg = P.ps("lg", [128, 16])
    lgs = [P.sb("lgs%d" % i, [128, 16]) for i in range(2)]
    mx = [P.sb("mx%d" % i, [128, 1]) for i in range(2)]
    sm = [P.sb("sm%d" % i, [128, 1]) for i in range(2)]
    for t in range(NT):
        b2 = t % 2
        rows = slice(t * 128, (t + 1) * 128)
        P.dma("sp", mts[b2][:], mix[rows, :])
        P.dma("sp", xts[b2][:], x[rows, :])
        P.dma("act", mgs[b2][:], mg[t:t + 1, :].partition_broadcast(128))
        P.dma("act", scs[b2][:], msc[t:t + 1, :].partition_broadcast(128))
        P.dma("act", shs[b2][:], msh[t:t + 1, :].partition_broadcast(128))
        P.copy("pool", mb[b2][:], mts[b2][:])
        for k in range(8):
            P.tr(tp[b2][:, k * 128:(k + 1) * 128], mb[b2][:, k * 128:(k + 1) * 128], ident[:])
        P.copy("act", mT[b2][:], tp[b2][:])
        xt = xts[b2]
        for n in range(2):
            acc = accs[n]
            for k in range(8):
                P.mm(acc[:], mT[b2][:, k * 128:(k + 1) * 128], wb[:, k, n * 512:(n + 1) * 512], start=(k == 0), stop=(k == 7))
            sl = slice(n * 512, (n + 1) * 512)
            P.tt("dve", mts[b2][:, sl], acc[:], mgs[b2][:, sl], ALU.mult)
        P.tt("pool", xt[:], xt[:], mts[b2][:], ALU.add)
        P.dma("pool", x1o[rows, :], xt[:])
        P.actf(junk[:], xt[:], AF.Square, accum=ssq[b2][:])
        P.ts("dve", rstd[b2][:], ssq[b2][:], 1.0 / D, ALU.mult, EPS, ALU.add)
        P.actf(rstd[b2][:], rstd[b2][:], AF.Sqrt)
        P.op("dve", lambda e, o=rstd[b2]: e.reciprocal(out=o[:], in_=o[:]), [rstd[b2]], [rstd[b2]])
        P.ts("pool", scs[b2][:], scs[b2][:], 1.0, ALU.add)
        P.tt("pool", scs[b2][:], scs[b2][:], gbc[:], ALU.mult)
        P.stt("dve", h32[b2][:], xt[:], rstd[b2][:], scs[b2][:], ALU.mult, ALU.mult)
        P.tt("pool", h32[b2][:], h32[b2][:], shs[b2][:], ALU.add)
        P.copy("act", hb[b2][:], h32[b2][:])
        P.dma("pool", h2o[rows, :], hb[b2][:])
        for half in range(2):
            for k in range(4):
                kk = half * 4 + k
                P.tr(tpf[half][:, k * 128:(k + 1) * 128], h32[b2][:, kk * 128:(kk + 1) * 128], identf[:])
            P.copy("act" if half == 0 else "dve", hTf[b2][:, half * 512:(half + 1) * 512], tpf[half][:])
        for k in range(8):
            P.mm(lg[:], hTf[b2][:, k * 128:(k + 1) * 128], rwt[:, k, :], start=(k == 0), stop=(k == 7))
        P.op("dve", lambda e, o=mx[b2]: e.reduce_max(out=o[:], in_=lg[:], axis=AX.X), [lg], [mx[b2]])
        P.ts("dve", mx[b2][:], mx[b2][:], -1.0, ALU.mult)
        P.actf(lgs[b2][:], lg[:], AF.Exp, bias=mx[b2][:], accum=sm[b2][:])
        P.op("dve", lambda e, o=sm[b2]: e.reciprocal(out=o[:], in_=o[:]), [sm[b2]], [sm[b2]])
        P.ts("dve", lgs[b2][:], lgs[b2][:], sm[b2][:], ALU.mult)
        P.dma("pool", affo[rows, :], lgs[b2][:])
    return P


IDENTF = np.eye(128, dtype=np.float32)


def run_tok_c(mix, x, mods_i, g2, w_out, router):
    mg, msh, msc = modrows(mods_i, 2), modrows(mods_i, 3), modrows(mods_i, 4)
    maps = []
    for core in range(NCORES):
        r = slice(core * TPC, (core + 1) * TPC)
        maps.append({"mix": mix[r], "x": x[r], "mg": mg[core], "msh": msh[core], "msc": msc[core],
                     "g": g2[None, :], "w": w_out, "rw": router, "ident": IDENT, "identf": IDENTF})
    res = launch("tok_c", build_tok_c, maps)
    cat = lambda k: np.concatenate([res[c][k] for c in range(NCORES)], 0)
    return cat("x1o"), cat("h2o"), cat("affo")


def build_topk():
    P = Prog()
    aff = P.dram("aff", [8, SEQ + CTXL])
    out = P.dram("gt", [8, SEQ + CTXL], kind="ExternalOutput")
    a = P.sb("a", [8, SEQ + CTXL])
    wk = P.sb("wk", [8, SEQ + CTXL])
    m8 = P.sb("m8", [8, 8])
    P.dma("sp", a[:], aff)
    P.copy("dve", wk[:], a[:])
    for (lo, n, cap) in ((0, SEQ, 2 * SEQ // 16), (SEQ, CTXL, 2 * CTXL // 16)):
        seg = wk[:, lo:lo + n]
        for r in range(cap // 8):
            P.op("dve", lambda e, seg=seg: e.max(out=m8[:], in_=seg), [wk], [m8])
            P.op("dve", lambda e, seg=seg: e.match_replace(out=seg, in_to_replace=m8[:], in_values=seg, imm_value=0.0), [wk, m8], [wk])
    P.tt("dve", wk[:], a[:], wk[:], ALU.subtract)
    P.dma("sp", out, wk[:])
    return P


def run_topk(aff):
    al = aff[:B * SEQ].reshape(B, SEQ, 16)
    ac = aff[B * SEQ:].reshape(B, CTXL, 16)
    maps = []
    for core in range(NCORES):
        b, hf = core // 2, core % 2
        at = np.concatenate([al[b, :, hf * 8:(hf + 1) * 8].T, ac[b, :, hf * 8:(hf + 1) * 8].T], 1)
        maps.append({"aff": np.ascontiguousarray(at)})
    res = launch("topk", build_topk, maps)
    G = np.zeros((NTOK, 16), np.float32)
    for core in range(NCORES):
        b, hf = core // 2, core % 2
        gt = res[core]["gt"]
        G[b * SEQ:(b + 1) * SEQ, hf * 8:(hf + 1) * 8] = gt[:, :SEQ].T
        G[B * SEQ + b * CTXL:B * SEQ + (b + 1) * CTXL, hf * 8:(hf + 1) * 8] = gt[:, SEQ:].T
    return G


def build_moe():
    P = Prog()
    hT = P.dram("hT", [D, TPC], BF16)
    G = P.dram("G", [TPC, 16])
    w1 = P.dram("w1", [16, D, D])
    w3 = P.dram("w3", [16, D, D])
    w2 = P.dram("w2", [16, D, D])
    yo = P.dram("y", [TPC, D], kind="ExternalOutput")
    hs = P.sb("hs", [128, 8, TPC], BF16)
    for k in range(8):
        P.dma("sp" if k % 2 == 0 else "act", hs[:, k, :], hT[k * 128:(k + 1) * 128, :])
    Gs = P.sb("Gs", [128, NT, 16])
    P.dma("sp", Gs[:], G.rearrange("(t p) e -> p t e", p=128))
    yacc = P.sb("yacc", [128, NT, D])
    wb = {nm: P.sb(nm + "b", [128, 8, D], BF16) for nm in ("w1", "w3", "w2")}
    wsrc = {"w1": w1, "w3": w3, "w2": w2}
    wst = [P.sb("wst%d" % i, [128, D]) for i in range(4)]
    hact = P.sb("hact", [128, 8, 512], BF16)
    sA = [P.sb("sA%d" % i, [128, 512]) for i in range(2)]
    pb = [P.ps("pb%d" % i, [128, 512]) for i in range(8)]
    chunks = [(c0, min(512, TPC - c0)) for c0 in range(0, TPC, 512)]
    si = 0
    cast_engs = ("dve", "pool", "act")
    for e in range(16):
        for nm in ("w1", "w3", "w2"):
            for k in range(8):
                st = wst[si % 4]
                P.dma("sp" if si % 2 == 0 else "act", st[:], wsrc[nm][e, k * 128:(k + 1) * 128, :])
                P.copy(cast_engs[si % 3], wb[nm][:, k, :], st[:])
                si += 1
        for (c0, cn) in chunks:
            for fc in range(8):
                A = pb[(fc % 2) * 2]
                Bm = pb[(fc % 2) * 2 + 1]
                for kd in range(8):
                    P.mm(A[:, 0:cn], wb["w1"][:, kd, fc * 128:(fc + 1) * 128], hs[:, kd, c0:c0 + cn], start=(kd == 0), stop=(kd == 7))
                for kd in range(8):
                    P.mm(Bm[:, 0:cn], wb["w3"][:, kd, fc * 128:(fc + 1) * 128], hs[:, kd, c0:c0 + cn], start=(kd == 0), stop=(kd == 7))
                P.actf(sA[fc % 2][:, 0:cn], A[:, 0:cn], AF.Silu)
                P.tt("dve", hact[:, fc, 0:cn], sA[fc % 2][:, 0:cn], Bm[:, 0:cn], ALU.mult)
            for tt in range(cn // 128):
                tile = c0 // 128 + tt
                for n2 in range(2):
                    acc = pb[4 + (tt * 2 + n2) % 4]
                    for fc in range(8):
                        P.mm(acc[:], hact[:, fc, tt * 128:(tt + 1) * 128], wb["w2"][:, fc, n2 * 512:(n2 + 1) * 512], start=(fc == 0), stop=(fc == 7))
                    ysl = yacc[:, tile, n2 * 512:(n2 + 1) * 512]
                    if e == 0:
                        P.ts("dve", ysl, acc[:], Gs[:, tile, e:e + 1], ALU.mult)
                    else:
                        P.stt("dve", ysl, acc[:], Gs[:, tile, e:e + 1], ysl, ALU.mult, ALU.add)
    for t in range(NT):
        P.dma("sp" if t % 2 == 0 else "pool", yo[t * 128:(t + 1) * 128, :], yacc[:, t, :])
    return P


def run_moe(h2, G, w1, w3, w2):
    maps = []
    for core in range(NCORES):
        r = slice(core * TPC, (core + 1) * TPC)
        maps.append({"hT": np.ascontiguousarray(h2[r].T), "G": G[r], "w1": w1, "w3": w3, "w2": w2})
    res = launch("moe", build_moe, maps)
    return np.concatenate([res[c]["y"] for c in range(NCORES)], 0)


NKEY = SEQ + CTXL
NQ = SEQ // 2


def build_attn(lam_init, NQ=SEQ // 2, NKEY=SEQ + CTXL):
    P = Prog()
    QB = min(512, NQ)
    NQT = QB // 128
    NKC = NKEY // 128
    qT = P.dram("qT", [16, 64, NQ], BF16)
    kT = P.dram("kT", [16, 64, NKEY], BF16)
    va = P.dram("va", [8, NKEY, 132], BF16)
    lamv = P.dram("lamv", [1, 256])
    sg = P.dram("sg", [1, 128])
    out = P.dram("o", [NQ, 8 * 128], kind="ExternalOutput")
    lt = P.sb("lt", [128, 256])
    P.dma("sp", lt[:], lamv.partition_broadcast(128))
    sgb = P.sb("sgb", [128, 128])
    P.dma("sp", sgb[:], sg.partition_broadcast(128))
    P.ts("dve", sgb[:], sgb[:], 1.0 - lam_init, ALU.mult)
    lp = P.sb("lp", [128, 128])
    ls = P.sb("ls", [128, 2])
    lv = lt[:].rearrange("p (a d) -> p a d", a=4)
    P.tt("dve", lp[:, 0:64], lv[:, 0, :], lv[:, 1, :], ALU.mult)
    P.tt("dve", lp[:, 64:128], lv[:, 2, :], lv[:, 3, :], ALU.mult)
    P.op("dve", lambda e: e.reduce_sum(out=ls[:], in_=lp[:].rearrange("p (a d) -> p a d", a=2), axis=AX.X), [lp], [ls])
    P.actf(ls[:], ls[:], AF.Exp)
    nlam = P.sb("nlam", [128, 1])
    P.tt("dve", nlam[:], ls[:, 1:2], ls[:, 0:1], ALU.subtract)
    P.ts("dve", nlam[:], nlam[:], -lam_init, ALU.add)

    qs = [P.sb("qs%d" % i, [64, 2, NQ], BF16) for i in range(2)]
    ks = [P.sb("ks%d" % i, [64, 2, NKEY], BF16) for i in range(2)]
    vs = [P.sb("vs%d" % i, [128, NKC, 132], BF16) for i in range(2)]
    sps = [P.ps("sp%d" % i, [128, 512]) for i in range(2)]
    ops_ = [P.ps("op%d" % i, [128, 512]) for i in range(4)]
    es = [P.sb("es%d" % i, [128, 512], BF16) for i in range(3)]
    o0 = [P.sb("o0_%d" % i, [128, 4, 128]) for i in range(2)]
    rz = [P.sb("rz%d" % i, [128, 4]) for i in range(2)]
    o1 = P.sb("o1", [128, 128])
    junk = P.sb("junk", [128, 128])
    ss = P.sb("ss", [128, 1])
    ob = [P.sb("ob%d" % i, [128, 8 * 128]) for i in range(4)]
    ei = 0
    si = 0
    for qb in range(NQ // QB):
        for h in range(8):
            hb_ = (qb * 8 + h) % 2
            P.dma("sp", qs[hb_][:], qT[2 * h:2 * h + 2, :, :].rearrange("c d q -> d c q"))
            P.dma("act", ks[hb_][:], kT[2 * h:2 * h + 2, :, :].rearrange("c d k -> d c k"))
            P.dma("sp", vs[hb_][:], va[h].rearrange("(n p) e -> p n e", p=128))
            for c in range(2):
                for kc in range(NKC):
                    spb = sps[si % 2]
                    si += 1
                    P.mm(spb[:, 0:QB], ks[hb_][:, c, kc * 128:(kc + 1) * 128], qs[hb_][:, c, qb * QB:(qb + 1) * QB])
                    eb = es[ei % 3]
                    ei += 1
                    P.actf(eb[:, 0:QB], spb[:, 0:QB], AF.Exp, scale=0.125)
                    for qt in range(NQT):
                        P.mm(ops_[qt][:, 0:132], eb[:, qt * 128:(qt + 1) * 128], vs[hb_][:, kc, :], start=(kc == 0), stop=(kc == NKC - 1))
                for qt in range(NQT):
                    if c == 0:
                        P.op("dve", lambda e, qt=qt, r=rz[0]: e.reciprocal(out=r[:, qt:qt + 1], in_=ops_[qt][:, 128:129]), [ops_[qt]], [rz[0]])
                        P.ts("dve", o0[0][:, qt, :], ops_[qt][:, 0:128], rz[0][:, qt:qt + 1], ALU.mult)
                    else:
                        P.op("dve", lambda e, qt=qt, r=rz[1]: e.reciprocal(out=r[:, qt:qt + 1], in_=ops_[qt][:, 128:129]), [ops_[qt]], [rz[1]])
                        P.ts("dve", o1[:], ops_[qt][:, 0:128], rz[1][:, qt:qt + 1], ALU.mult)
                        P.stt("dve", o1[:], o1[:], nlam[:], o0[0][:, qt, :], ALU.mult, ALU.add)
                        P.actf(junk[:], o1[:], AF.Square, accum=ss[:])
                        P.ts("dve", ss[:], ss[:], 1.0 / 128, ALU.mult, 1e-5, ALU.add)
                        P.actf(ss[:], ss[:], AF.Sqrt)
                        P.op("dve", lambda e: e.reciprocal(out=ss[:], in_=ss[:]), [ss], [ss])
                        P.stt("dve", ob[qt][:, h * 128:(h + 1) * 128], o1[:], ss[:], sgb[:], ALU.mult, ALU.mult)
        for qt in range(NQT):
            r0 = qb * QB + qt * 128
            P.dma("pool", out[r0:r0 + 128, :], ob[qt][:])
    return P


def run_attn(p, da_lambda, da_subln, lam_init):
    pl = p[:B * SEQ].reshape(B, SEQ, 3072)
    pc = p[B * SEQ:].reshape(B, CTXL, 3072)
    maps = []
    for core in range(NCORES):
        b, hf = core // 2, core % 2
        q = pl[b, hf * NQ:(hf + 1) * NQ, 0:1024].reshape(NQ, 16, 64)
        kk = np.concatenate([pc[b, :, 1024:2048], pl[b, :, 1024:2048]], 0).reshape(NKEY, 16, 64)
        v = np.concatenate([pc[b, :, 2048:3072], pl[b, :, 2048:3072]], 0).reshape(NKEY, 8, 128)
        va = np.zeros((8, NKEY, 132), NPBF)
        va[:, :, :128] = v.transpose(1, 0, 2)
        va[:, :, 128] = 1.0
        maps.append({"qT": np.ascontiguousarray(q.transpose(1, 2, 0)), "kT": np.ascontiguousarray(kk.transpose(1, 2, 0)),
                     "va": va, "lamv": np.ascontiguousarray(da_lambda.reshape(1, 256)), "sg": da_subln[None, :]})
    res = launch(("attn", lam_init), lambda: build_attn(lam_init), maps)
    o = np.zeros((NTOK, 1024), np.float32)
    for core in range(NCORES):
        b, hf = core // 2, core % 2
        o[b * SEQ + hf * NQ:b * SEQ + (hf + 1) * NQ] = res[core]["o"]
    return o


def run_attn_ctx(p, da_lambda, da_subln, lam_init):
    pc = p[B * SEQ:].reshape(B, CTXL, 3072)
    maps = []
    for core in range(NCORES):
        b, hf = core // 2, core % 2
        q = pc[b, hf * 128:(hf + 1) * 128, 0:1024].reshape(128, 16, 64)
        kk = pc[b, :, 1024:2048].reshape(CTXL, 16, 64)
        v = pc[b, :, 2048:3072].reshape(CTXL, 8, 128)
        va = np.zeros((8, CTXL, 132), NPBF)
        va[:, :, :128] = v.transpose(1, 0, 2)
        va[:, :, 128] = 1.0
        maps.append({"qT": np.ascontiguousarray(q.transpose(1, 2, 0)), "kT": np.ascontiguousarray(kk.transpose(1, 2, 0)),
                     "va": va, "lamv": np.ascontiguousarray(da_lambda.reshape(1, 256)), "sg": da_subln[None, :]})
    res = launch(("attn_ctx", lam_init), lambda: build_attn(lam_init, 128, CTXL), maps)
    o = np.zeros((B * CTXL, 1024), np.float32)
    for core in range(NCORES):
        b, hf = core // 2, core % 2
        o[b * CTXL + hf * 128:b * CTXL + (hf + 1) * 128] = res[core]["o"]
    return o


RWC = 1760
QC = 10 * 512 + 8


def seg_shift(p, k):
    out = np.zeros_like(p)
    segs = [(b * SEQ, SEQ) for b in range(B)] + [(B * SEQ + b * CTXL, CTXL) for b in range(B)]
    for (s0, n) in segs:
        if k == 1:
            out[s0 + 1:s0 + n] = p[s0:s0 + n - 1]
        else:
            out[s0:s0 + n - 1] = p[s0 + 1:s0 + n]
    return out


def build_rwprep():
    P = Prog()
    pc = P.dram("pc", [TPC, RWC])
    pp = P.dram("pp", [TPC, RWC])
    pn = P.dram("pn", [TPC, RWC])
    mu = P.dram("mu", [2, RWC])
    w0 = P.dram("w0", [2, 512])
    a0 = P.dram("a0", [2, 512])
    vec = P.dram("vec", [3, 512])
    lw_d = P.dram("lw", [128, 2048])
    lg_d = P.dram("lg", [128, 512])
    identf_d = P.dram("identf", [128, 128])
    qo = P.dram("q", [TPC, QC], kind="ExternalOutput")
    identf = P.sb("identfs", [128, 128])
    P.dma("sp", identf[:], identf_d)
    mub = [P.sb("mub%d" % i, [128, RWC]) for i in range(2)]
    w0b = [P.sb("w0b%d" % i, [128, 512]) for i in range(2)]
    a0b = [P.sb("a0b%d" % i, [128, 512]) for i in range(2)]
    vb = [P.sb("vb%d" % i, [128, 512]) for i in range(3)]
    for i in range(2):
        P.dma("sp", mub[i][:], mu[i:i + 1, :].partition_broadcast(128))
        P.dma("act", w0b[i][:], w0[i:i + 1, :].partition_broadcast(128))
        P.dma("act", a0b[i][:], a0[i:i + 1, :].partition_broadcast(128))
    for i in range(3):
        P.dma("sp", vb[i][:], vec[i:i + 1, :].partition_broadcast(128))
    lw = P.sb("lws", [128, 2048])
    lg = P.sb("lgs", [128, 512])
    P.dma("sp", lw[:], lw_d)
    P.dma("sp", lg[:], lg_d)
    X = P.sb("X", [128, 256])
    P.op("dve", lambda e: e.memset(X[:], 0.0), [], [X])
    pcs = [P.sb("pcs%d" % i, [128, RWC]) for i in range(2)]
    pps = [P.sb("pps%d" % i, [128, RWC]) for i in range(2)]
    pns = [P.sb("pns%d" % i, [128, RWC]) for i in range(2)]
    qt = [P.sb("qt%d" % i, [128, QC]) for i in range(2)]
    tpA = P.ps("tpA", [128, 256])
    XT = P.sb("XT", [128, 256])
    lin = [P.ps("lin%d" % i, [128, 512]) for i in range(5)]
    tw = [P.sb("tw%d" % i, [128, 512]) for i in range(2)]
    ad = [P.sb("ad%d" % i, [128, 512]) for i in range(2)]
    kr = P.sb("kr", [128, 512])
    sq = P.sb("sq", [128, 512])
    s8 = P.sb("s8", [128, 8])
    t5 = P.sb("t5", [128, 512])
    for t in range(NT):
        b2 = t % 2
        rows = slice(t * 128, (t + 1) * 128)
        u, up, un, q = pcs[b2], pps[b2], pns[b2], qt[b2]
        P.dma("sp", u[:], pc[rows, :])
        P.dma("act", up[:], pp[rows, :])
        P.dma("sp", un[:], pn[rows, :])
        P.tt("pool", up[:], up[:], u[:], ALU.subtract)
        P.tt("pool", up[:], up[:], mub[0][:], ALU.mult)
        P.tt("dve", un[:], un[:], u[:], ALU.subtract)
        P.tt("dve", un[:], un[:], mub[1][:], ALU.mult)
        P.tt("pool", u[:], u[:], up[:], ALU.add)
        P.tt("dve", u[:], u[:], un[:], ALU.add)
        r_, k_, v_ = u[:, 0:512], u[:, 512:1024], u[:, 1024:1536]
        P.copy("pool", q[:, 0:512], r_)
        P.copy("pool", q[:, 512:1024], v_)
        P.actf(X[:, 0:64], u[:, 1536:1600], AF.Tanh)
        P.copy("pool", X[:, 64:128], u[:, 1600:1664])
        P.actf(X[:, 128:224], u[:, 1664:1760], AF.Sigmoid)
        P.tr(tpA[:, 0:128], X[:, 0:128], identf[:])
        P.tr(tpA[:, 128:256], X[:, 128:256], identf[:])
        P.copy("act", XT[:], tpA[:])
        for i in range(4):
            P.mm(lin[i][:], XT[:, 0:128], lw[:, i * 512:(i + 1) * 512])
        P.mm(lin[4][:], XT[:, 128:256], lg[:])
        P.copy("act", q[:, 9 * 512:10 * 512], lin[4][:])
        P.tt("pool", kr[:], k_, vb[0][:], ALU.mult)
        P.tt("pool", sq[:], kr[:], kr[:], ALU.mult)
        P.op("dve", lambda e: e.reduce_sum(out=s8[:], in_=sq[:].rearrange("p (h j) -> p h j", h=8), axis=AX.X), [sq], [s8])
        P.actf(s8[:], s8[:], AF.Sqrt)
        P.ts("dve", s8[:], s8[:], 1e-12, ALU.max)
        P.op("dve", lambda e: e.reciprocal(out=s8[:], in_=s8[:]), [s8], [s8])
        kk3 = q[:, 2 * 512:3 * 512].rearrange("p (h j) -> p h j", h=8)
        P.tt("dve", kk3, kr[:].rearrange("p (h j) -> p h j", h=8), s8[:].unsqueeze(2).broadcast_to([128, 8, 64]), ALU.mult)
        for d in range(2):
            P.tt("dve", tw[d][:], lin[d][:], w0b[d][:], ALU.add)
            P.actf(tw[d][:], tw[d][:], AF.Sigmoid)
            P.actf(q[:, (3 + d) * 512:(4 + d) * 512], tw[d][:], AF.Exp, scale=-0.6065306597126334)
            P.tt("dve", ad[d][:], lin[2 + d][:], a0b[d][:], ALU.add)
            P.actf(ad[d][:], ad[d][:], AF.Sigmoid)
            P.stt("dve", t5[:], ad[d][:], -1.0, vb[1][:], ALU.add, ALU.mult)
            P.stt("dve", q[:, (5 + d) * 512:(6 + d) * 512], t5[:], 1.0, k_, ALU.add, ALU.mult)
            P.tt("pool", q[:, (7 + d) * 512:(8 + d) * 512], q[:, 2 * 512:3 * 512], ad[d][:], ALU.mult)
        P.tt("pool", t5[:], q[:, 5 * 512:6 * 512], q[:, 6 * 512:7 * 512], ALU.add)
        P.tt("pool", t5[:], t5[:], r_, ALU.mult)
        P.tt("pool", t5[:], t5[:], vb[2][:], ALU.mult)
        P.op("dve", lambda e, q=q: e.reduce_sum(out=q[:, 5120:5128], in_=t5[:].rearrange("p (h j) -> p h j", h=8), axis=AX.X), [t5], [q])
        P.dma("pool", qo[rows, :], q[:])
    return P


def run_rwprep(p, mu, w0, w2, a0, a2, g2, k_k, k_a, r_k):
    pcen = np.ascontiguousarray(p[:, :RWC])
    pprev, pnext = seg_shift(pcen, 1), seg_shift(pcen, -1)
    vec = np.stack([k_k, k_a, r_k.reshape(-1)], 0)
    LW = np.zeros((128, 2048), np.float32)
    for i, m_ in enumerate((w2[0], w2[1], a2[0], a2[1])):
        LW[i * 32:(i + 1) * 32, i * 512:(i + 1) * 512] = m_
    LG = np.zeros((128, 512), np.float32)
    LG[:96] = g2
    maps = []
    for core in range(NCORES):
        r = slice(core * TPC, (core + 1) * TPC)
        maps.append({"pc": pcen[r], "pp": pprev[r], "pn": pnext[r], "mu": mu, "w0": w0, "a0": a0, "vec": vec,
                     "lw": LW, "lg": LG, "identf": IDENTF})
    res = launch("rwprep", build_rwprep, maps)
    return np.concatenate([res[c]["q"] for c in range(NCORES)], 0)


NSTEP = SEQ + CTXL
NS = 8


def build_rwscan():
    P = Prog()
    rows = P.dram("rows", [2, NSTEP, 1280])
    vd = P.dram("vd", [128, NSTEP * 4])
    E_d = P.dram("E", [2, 128])
    yo = P.dram("y", [128, NSTEP * 4], kind="ExternalOutput")
    E = P.sb("Es", [2, 128])
    P.dma("sp", E[:], E_d)
    S = P.sb("S", [128, 256])
    P.op("dve", lambda e: e.memset(S[:], 0.0), [], [S])
    rb = [P.sb("rb%d" % i, [2, NS, 1280]) for i in range(2)]
    vb = [P.sb("vb%d" % i, [128, NS * 4]) for i in range(2)]
    yb = [P.sb("yb%d" % i, [128, NS * 4]) for i in range(2)]
    bc = [[P.ps("bc%d_%d" % (i, j), [128, 512]) for j in range(3)] for i in range(2)]
    t1 = P.sb("t1", [128, 256])
    t2 = P.sb("t2", [128, 256])
    t3 = P.sb("t3", [128, 256])
    t4 = P.sb("t4", [128, 256])
    sa = P.sb("sa", [128, 4])
    v3 = lambda ap: ap.rearrange("p (b j) -> p b j", b=4)
    for ch in range(NSTEP // NS):
        b2 = ch % 2
        P.dma("sp", rb[b2][:], rows[:, ch * NS:(ch + 1) * NS, :])
        P.dma("act", vb[b2][:], vd[:, ch * NS * 4:(ch + 1) * NS * 4])
        for s in range(NS):
            n = ch * NS + s
            pbk = bc[n % 2]
            P.mm(pbk[0][:], E[:], rb[b2][:, s, 0:512])
            P.mm(pbk[1][:], E[:], rb[b2][:, s, 512:1024])
            P.mm(pbk[2][:, 0:256], E[:], rb[b2][:, s, 1024:1280])
            KK, W, BE, KD, R = pbk[0][:, 0:256], pbk[0][:, 256:512], pbk[1][:, 0:256], pbk[1][:, 256:512], pbk[2][:, 0:256]
            vcol = vb[b2][:, s * 4:(s + 1) * 4]
            P.tt("dve", t1[:], S[:], KK, ALU.mult)
            P.op("dve", lambda e: e.reduce_sum(out=sa[:], in_=v3(t1[:]), axis=AX.X), [t1], [sa])
            P.tt("dve", S[:], S[:], W, ALU.mult)
            P.tt("dve", v3(t2[:]), v3(BE), sa[:].unsqueeze(2).broadcast_to([128, 4, 64]), ALU.mult)
            P.tt("dve", S[:], S[:], t2[:], ALU.subtract)
            P.tt("dve", v3(t3[:]), v3(KD), vcol.unsqueeze(2).broadcast_to([128, 4, 64]), ALU.mult)
            P.tt("dve", S[:], S[:], t3[:], ALU.add)
            P.tt("dve", t4[:], S[:], R, ALU.mult)
            P.op("dve", lambda e, yt=yb[b2], s=s: e.reduce_sum(out=yt[:, s * 4:(s + 1) * 4], in_=v3(t4[:]), axis=AX.X), [t4], [yb[b2]])
        P.dma("pool", yo[:, ch * NS * 4:(ch + 1) * NS * 4], yb[b2][:])
    return P


def scan_order():
    idx = np.zeros((2, NSTEP, B), np.int64)
    for b in range(B):
        cf = B * SEQ + b * CTXL + np.arange(CTXL)
        lf = b * SEQ + np.arange(SEQ)
        idx[0, :, b] = np.concatenate([cf, lf])
        idx[1, :, b] = np.concatenate([cf[::-1], lf[::-1]])
    return idx


def run_rwscan(q):
    idx = scan_order()
    col = lambda k, h: slice(k * 512 + h * 64, k * 512 + (h + 1) * 64)
    Emat = np.zeros((2, 128), np.float32)
    Emat[0, :64] = 1.0
    Emat[1, 64:] = 1.0
    maps = []
    for h in range(NCORES):
        rows = np.zeros((2, NSTEP, 5, B, 64), np.float32)
        vd = np.zeros((2, 64, NSTEP, B), np.float32)
        for d in range(2):
            ii = idx[d]
            for oi, k in enumerate((2, 3 + d, 7 + d, 5 + d, 0)):
                rows[d, :, oi] = q[ii][:, :, col(k, h)]
            vd[d] = q[ii][:, :, col(1, h)].transpose(2, 0, 1)
        maps.append({"rows": rows.reshape(2, NSTEP, 1280), "vd": vd.reshape(128, NSTEP * 4), "E": Emat})
    res = launch("rwscan", build_rwscan, maps)
    ys = [np.zeros((NTOK, 512), np.float32) for _ in range(2)]
    for h in range(NCORES):
        y = res[h]["y"].reshape(2, 64, NSTEP, B)
        for d in range(2):
            for b in range(B):
                ys[d][idx[d][:, b], h * 64:(h + 1) * 64] = y[d, :, :, b].T
    return ys


def build_rwout():
    P = Prog()
    yf = P.dram("yf", [TPC, 512])
    ybk = P.dram("yb", [TPC, 512])
    q = P.dram("q", [TPC, QC])
    gn = P.dram("gn", [2, 512])
    out = P.dram("rw", [TPC, 512], kind="ExternalOutput")
    gnb = [P.sb("gnb%d" % i, [128, 512]) for i in range(2)]
    for i in range(2):
        P.dma("sp", gnb[i][:], gn[i:i + 1, :].partition_broadcast(128))
    a = [P.sb("a%d" % i, [128, 512]) for i in range(2)]
    bq = [P.sb("bq%d" % i, [128, 512]) for i in range(2)]
    vv = [P.sb("vv%d" % i, [128, 512]) for i in range(2)]
    gg = [P.sb("gg%d" % i, [128, 512]) for i in range(2)]
    bo = [P.sb("bo%d" % i, [128, 8]) for i in range(2)]
    m8 = P.sb("m8", [128, 8])
    sq = P.sb("sq", [128, 512])
    ot = [P.sb("ot%d" % i, [128, 512]) for i in range(2)]
    h3 = lambda ap: ap.rearrange("p (h j) -> p h j", h=8)
    b3 = lambda ap: ap.unsqueeze(2).broadcast_to([128, 8, 64])
    for t in range(NT):
        b2 = t % 2
        rows = slice(t * 128, (t + 1) * 128)
        P.dma("sp", a[b2][:], yf[rows, :])
        P.dma("act", bq[b2][:], ybk[rows, :])
        P.dma("sp", vv[b2][:], q[rows, 512:1024])
        P.dma("act", gg[b2][:], q[rows, 4608:5120])
        P.dma("sp", bo[b2][:], q[rows, 5120:5128])
        y = a[b2]
        P.tt("dve", y[:], y[:], bq[b2][:], ALU.add)
        P.op("dve", lambda e, y=y: e.reduce_sum(out=m8[:], in_=h3(y[:]), axis=AX.X), [y], [m8])
        P.ts("dve", m8[:], m8[:], -1.0 / 64, ALU.mult)
        P.tt("dve", h3(y[:]), h3(y[:]), b3(m8[:]), ALU.add)
        P.tt("pool", sq[:], y[:], y[:], ALU.mult)
        P.op("dve", lambda e: e.reduce_sum(out=m8[:], in_=h3(sq[:]), axis=AX.X), [sq], [m8])
        P.ts("dve", m8[:], m8[:], 1.0 / 64, ALU.mult, 64e-5, ALU.add)
        P.actf(m8[:], m8[:], AF.Sqrt)
        P.op("dve", lambda e: e.reciprocal(out=m8[:], in_=m8[:]), [m8], [m8])
        P.tt("dve", h3(y[:]), h3(y[:]), b3(m8[:]), ALU.mult)
        P.tt("pool", y[:], y[:], gnb[0][:], ALU.mult)
        P.tt("pool", y[:], y[:], gnb[1][:], ALU.add)
        P.tt("dve", h3(vv[b2][:]), h3(vv[b2][:]), b3(bo[b2][:]), ALU.mult)
        P.tt("dve", y[:], y[:], vv[b2][:], ALU.add)
        P.tt("pool", ot[b2][:], y[:], gg[b2][:], ALU.mult)
        P.dma("pool", out[rows, :], ot[b2][:])
    return P


def run_rwout(yf, yb, q, gn):
    maps = []
    for core in range(NCORES):
        r = slice(core * TPC, (core + 1) * TPC)
        maps.append({"yf": yf[r], "yb": yb[r], "q": q[r], "gn": gn})
    res = launch("rwout", build_rwout, maps)
    return np.concatenate([res[c]["rw"] for c in range(NCORES)], 0)


TWO_PI = 2.0 * math.pi


def build_hyena(L):
    P = Prog()
    KT = L // 128
    FT = KT + 1
    Fp = FT * 128
    NB = B * 64
    zT = P.dram("zT", [128, L])
    win = P.dram("win", [L, 64])
    tabC = P.dram("tabC", [L, Fp], BF16)
    tabS = P.dram("tabS", [L, Fp], BF16)
    tabCi = P.dram("tabCi", [Fp, L], BF16)
    tabSi = P.dram("tabSi", [Fp, L], BF16)
    cfn_d = P.dram("cfn", [128, FT])
    U3 = P.dram("U3", [3, L, 3 * NB])
    cw_d = P.dram("cw", [4, 3 * NB])
    fw = P.dram("fw", [128, 64 + 64 + 256])
    fc = P.dram("fc", [128, 4])
    hb_d = P.dram("hb", [2, NB])
    out = P.dram("hy", [L, NB], kind="ExternalOutput")

    cfn = P.sb("cfns", [128, FT])
    P.dma("sp", cfn[:], cfn_d)
    cwb = P.sb("cwb", [128, 4, 3 * NB])
    for i in range(4):
        P.dma("act", cwb[:, i, :], cw_d[i:i + 1, :].partition_broadcast(128))
    fws = P.sb("fws", [128, 384])
    P.dma("sp", fws[:], fw)
    fcs = P.sb("fcs", [128, 4])
    P.dma("sp", fcs[:], fc)
    fk = P.sb("fk", [128, 2])
    for li in range(2):
        P.ts("dve", fk[:, li:li + 1], fcs[:, 2 * li + 1:2 * li + 2], fcs[:, 2 * li:2 * li + 1], ALU.mult, 0.0, ALU.add)
    hbb = P.sb("hbb", [128, 2, NB])
    for i in range(2):
        P.dma("act", hbb[:, i, :], hb_d[i:i + 1, :].partition_broadcast(128))
    ones = P.sb("ones", [128, 128])
    P.op("dve", lambda e: e.memset(ones[:], 1.0), [], [ones])
    pb = [P.ps("pb%d" % i, [128, 512]) for i in range(8)]

    hs = P.sb("hs", [128, KT, 128], BF16)
    hd = P.sb("hd", [128, KT, 128], BF16)
    hh = P.sb("hh", [128, 1, 256])
    PC = min(512, L)
    zt = P.sb("zt", [128, PC])
    rr = P.sb("rr", [128, PC])
    ri = P.sb("ri", [128, PC], mybir.dt.int32)
    h1 = P.sb("h1", [128, PC])
    h2 = P.sb("h2", [128, PC])
    P.op("dve", lambda e: e.memset(h1[:], 0.0), [], [h1])
    P.op("dve", lambda e: e.memset(h2[:], 0.0), [], [h2])
    wn = P.sb("wn", [128, 64])
    ab = P.sb("ab", [128, 256])
    for pc0 in range(0, L, PC):
        P.dma("sp", zt[:], zT[:, pc0:pc0 + PC])
        for li, (hin, hout, wsl) in enumerate(((zt, h1, fws[:, 0:64]), (h1, h2, fws[:, 64:128]))):
            P.mm(pb[0][0:64, 0:PC], wsl, hin[:])
            P.ts("dve", hout[0:64, :], pb[0][0:64, 0:PC], fcs[0:64, 2 * li:2 * li + 1], ALU.mult, fk[0:64, li:li + 1], ALU.add)
            P.ts("dve", rr[0:64, :], hout[0:64, :], 1.0 / TWO_PI, ALU.mult)
            P.copy("dve", ri[0:64, :], rr[0:64, :])
            P.copy("dve", rr[0:64, :], ri[0:64, :])
            P.stt("dve", hout[0:64, :], rr[0:64, :], -TWO_PI, hout[0:64, :], ALU.mult, ALU.add)
            P.ts("dve", rr[0:64, :], hout[0:64, :], math.pi, ALU.is_gt)
            P.stt("dve", hout[0:64, :], rr[0:64, :], -TWO_PI, hout[0:64, :], ALU.mult, ALU.add)
            P.ts("dve", rr[0:64, :], hout[0:64, :], -math.pi, ALU.is_lt)
            P.stt("dve", hout[0:64, :], rr[0:64, :], TWO_PI, hout[0:64, :], ALU.mult, ALU.add)
            P.actf(hout[0:64, :], hout[0:64, :], AF.Sin)
        for j in range(PC // 128):
            kt = pc0 // 128 + j
            P.mm(pb[1][:, 0:256], h2[:, j * 128:(j + 1) * 128], fws[:, 128:384])
            P.dma("act", wn[:], win[kt * 128:(kt + 1) * 128, :])
            P.tt("dve", hh[:, 0, :].rearrange("p (a c) -> p a c", a=4), pb[1][:, 0:256].rearrange("p (a c) -> p a c", a=4),
                 wn[:].unsqueeze(1).broadcast_to([128, 4, 64]), ALU.mult)
            if kt == 0:
                hv = hh[0:1, 0, :].rearrange("p (o d c) -> p o d c", o=2, d=2)
                P.op("dve", lambda e, hv=hv: e.memset(hv[:, :, 1, :], 0.0), [hh], [hh])
            P.actf(ab[:], hh[:, 0, :], AF.Abs)
            P.mm(pb[2][:, 0:256], ones[:], ab[:], start=(kt == 0), stop=(kt == KT - 1))
            h4 = hh[:, 0, :].rearrange("p (o d c) -> p o d c", o=2, d=2)
            P.tt("dve", hs[:, kt, :].rearrange("p (o c) -> p o c", o=2), h4[:, :, 0, :], h4[:, :, 1, :], ALU.add)
            P.tt("pool", hd[:, kt, :].rearrange("p (o c) -> p o c", o=2), h4[:, :, 0, :], h4[:, :, 1, :], ALU.subtract)
    rn = P.sb("rn", [128, 128])
    n4 = pb[2][:, 0:256].rearrange("p (o d c) -> p o d c", o=2, d=2)
    nt = P.sb("nt", [128, 128])
    P.copy("act", nt[:].rearrange("p (o c) -> p o c", o=2), n4[:, :, 0, :])
    P.tt("dve", rn[:].rearrange("p (o c) -> p o c", o=2), nt[:].rearrange("p (o c) -> p o c", o=2), n4[:, :, 1, :], ALU.add)
    P.ts("dve", rn[:], rn[:], 1e-6, ALU.add)
    P.op("dve", lambda e: e.reciprocal(out=rn[:], in_=rn[:]), [rn], [rn])

    tb = [[P.sb("tb%d_%d" % (i, j), [128, 256], BF16) for j in range(2)] for i in range(4)]
    tbi = [0]

    def fwd_dft(rhsC, rhsS, N, epi):
        for g0 in range(0, FT, 2):
            nm = min(2, FT - g0)
            for k in range(KT):
                t = tb[tbi[0] % 4]
                tbi[0] += 1
                P.dma("sp", t[0][:, 0:nm * 128], tabC[k * 128:(k + 1) * 128, g0 * 128:(g0 + nm) * 128])
                P.dma("act", t[1][:, 0:nm * 128], tabS[k * 128:(k + 1) * 128, g0 * 128:(g0 + nm) * 128])
                for mi in range(nm):
                    P.mm(pb[mi * 2][:, 0:N], t[0][:, mi * 128:(mi + 1) * 128], rhsC[:, k, :], start=(k == 0), stop=(k == KT - 1))
                    P.mm(pb[mi * 2 + 1][:, 0:N], t[1][:, mi * 128:(mi + 1) * 128], rhsS[:, k, :], start=(k == 0), stop=(k == KT - 1))
            for mi in range(nm):
                epi(g0 + mi, pb[mi * 2][:, 0:N], pb[mi * 2 + 1][:, 0:N])

    def inv_dft(Yr, Ys, epi):
        for g0 in range(0, KT, 2):
            nm = min(2, KT - g0)
            for f in range(FT):
                t = tb[tbi[0] % 4]
                tbi[0] += 1
                P.dma("sp", t[0][:, 0:nm * 128], tabCi[f * 128:(f + 1) * 128, g0 * 128:(g0 + nm) * 128])
                P.dma("act", t[1][:, 0:nm * 128], tabSi[f * 128:(f + 1) * 128, g0 * 128:(g0 + nm) * 128])
                for mi in range(nm):
                    P.mm(pb[4 + mi][:, 0:NB], t[0][:, mi * 128:(mi + 1) * 128], Yr[:, f, :], start=(f == 0), stop=False)
                    P.mm(pb[4 + mi][:, 0:NB], t[1][:, mi * 128:(mi + 1) * 128], Ys[:, f, :], start=False, stop=(f == FT - 1))
            for mi in range(nm):
                epi(g0 + mi, pb[4 + mi][:, 0:NB])

    Kr = P.sb("Kr", [128, FT, 128])
    Ks = P.sb("Ks", [128, FT, 128])

    def epi_k(m, aC, aS):
        P.stt("dve", Kr[:, m, :], aC, cfn[:, m:m + 1], rn[:], ALU.mult, ALU.mult)
        P.stt("dve", Ks[:, m, :], aS, cfn[:, m:m + 1], rn[:], ALU.mult, ALU.mult)

    fwd_dft(hs, hd, 128, epi_k)

    V = P.sb("V", [128, KT, NB], BF16)
    X1 = P.sb("X1", [128, KT, NB], BF16)
    X2 = P.sb("X2", [128, KT, NB], BF16)
    Z = P.sb("Z", [128, KT, NB], BF16)
    us = [[P.sb("us%d_%d" % (i, j), [128, 3 * NB]) for j in range(3)] for i in range(2)]
    for kt in range(KT):
        b2 = kt % 2
        for sft in range(3):
            P.dma("sp" if sft != 1 else "act", us[b2][sft][:], U3[sft, kt * 128:(kt + 1) * 128, :])
        a0 = us[b2][1]
        P.tt("dve", a0[:], a0[:], cwb[:, 1, :], ALU.mult)
        P.tt("pool", us[b2][0][:], us[b2][0][:], cwb[:, 0, :], ALU.mult)
        P.tt("pool", us[b2][2][:], us[b2][2][:], cwb[:, 2, :], ALU.mult)
        P.tt("dve", a0[:], a0[:], us[b2][0][:], ALU.add)
        P.tt("dve", a0[:], a0[:], us[b2][2][:], ALU.add)
        P.tt("dve", a0[:], a0[:], cwb[:, 3, :], ALU.add)
        P.copy("act", V[:, kt, :], a0[:, 0:NB])
        P.copy("act", X1[:, kt, :], a0[:, NB:2 * NB])
        P.copy("pool", X2[:, kt, :], a0[:, 2 * NB:3 * NB])

    Yr = P.sb("Yr", [128, FT, NB], BF16)
    Ys = P.sb("Ys", [128, FT, NB], BF16)
    ta = P.sb("ta", [128, NB])
    tc_ = P.sb("tc", [128, NB])
    ur = P.sb("ur", [128, NB])
    usb = P.sb("usb", [128, NB])
    b4 = lambda ap: ap.rearrange("p (b c) -> p b c", b=B)

    def make_epi_y(o):
        kr = lambda m: Kr[:, m, o * 64:(o + 1) * 64].unsqueeze(1).broadcast_to([128, B, 64])
        ks = lambda m: Ks[:, m, o * 64:(o + 1) * 64].unsqueeze(1).broadcast_to([128, B, 64])

        def epi(m, aC, aS):
            P.copy("act", ur[:], aC)
            P.copy("act", usb[:], aS)
            P.tt("dve", b4(ta[:]), b4(ur[:]), kr(m), ALU.mult)
            P.tt("pool", b4(tc_[:]), b4(usb[:]), ks(m), ALU.mult)
            P.tt("dve", Yr[:, m, :], ta[:], tc_[:], ALU.subtract)
            P.tt("dve", b4(ta[:]), b4(ur[:]), ks(m), ALU.mult)
            P.tt("pool", b4(tc_[:]), b4(usb[:]), kr(m), ALU.mult)
            P.tt("dve", Ys[:, m, :], ta[:], tc_[:], ALU.add)
        return epi

    ot = [P.sb("ot%d" % i, [128, NB]) for i in range(2)]

    def epi_z(kt, acc):
        P.tt("dve", ta[:], V[:, kt, :], hbb[:, 0, :], ALU.mult)
        P.tt("dve", ta[:], ta[:], acc, ALU.add)
        P.tt("dve", Z[:, kt, :], ta[:], X1[:, kt, :], ALU.mult)

    def epi_o(kt, acc):
        o_ = ot[kt % 2]
        P.tt("dve", tc_[:], Z[:, kt, :], hbb[:, 1, :], ALU.mult)
        P.tt("dve", tc_[:], tc_[:], acc, ALU.add)
        P.tt("dve", o_[:], tc_[:], X2[:, kt, :], ALU.mult)
        P.dma("pool", out[kt * 128:(kt + 1) * 128, :], o_[:])

    fwd_dft(V, V, NB, make_epi_y(0))
    inv_dft(Yr, Ys, epi_z)
    fwd_dft(Z, Z, NB, make_epi_y(1))
    inv_dft(Yr, Ys, epi_o)
    return P


def negpi(P):
    if not hasattr(P, "_negpi"):
        t = P.sb("negpi", [128, 1])
        P.op("dve", lambda e: e.memset(t[:], -math.pi), [], [t])
        P._negpi = t
    return P._negpi[0:64, :]


_HY_CONST = {}


def hyena_consts(L):
    if L in _HY_CONST:
        return _HY_CONST[L]
    KT = L // 128
    Fp = (KT + 1) * 128
    f32 = np.float32
    t = np.linspace(0.0, 1.0, L, dtype=f32)[:, None]
    pos = np.arange(L, dtype=f32)[:, None]
    bands = np.linspace(1e-4, 15, 16, dtype=f32)[None, :]
    ang = (f32(2.0 * math.pi) * pos * bands / f32(L)).astype(f32)
    z = np.concatenate([t, np.cos(ang), -np.sin(ang)], -1).astype(f32)
    zT = np.zeros((128, L), f32)
    zT[:33] = z.T
    deltas = np.linspace(math.log(1e-2) / 1.5, math.log(1e-2) / 0.3, 512, dtype=f32)
    win = np.exp(-t * np.abs(deltas)[None, :]).astype(f32)
    a = np.arange(L, dtype=np.int64)[:, None]
    bq = np.arange(Fp, dtype=np.int64)[None, :]
    th = ((a * bq) % (2 * L)).astype(np.float64) * (math.pi / L)
    tabC = np.cos(th).astype(f32).astype(NPBF)
    tabS = np.sin(th).astype(f32).astype(NPBF)
    cf = np.zeros(Fp, f32)
    cf[:L + 1] = 2.0 / (2 * L)
    cf[0] = cf[L] = 1.0 / (2 * L)
    cfn = np.ascontiguousarray(cf.reshape(KT + 1, 128).T)
    _HY_CONST[L] = dict(zT=zT, win=win, tabC=tabC, tabS=tabS, tabCi=np.ascontiguousarray(tabC.T),
                        tabSi=np.ascontiguousarray(tabS.T), cfn=cfn)
    return _HY_CONST[L]


def run_hyena(pseg, L, conv_w, conv_b, f_w1, f_b1, f_w2, f_b2, f_w3, f_freq, hy_b):
    C = hyena_consts(L)
    prev = np.zeros_like(pseg)
    nxt = np.zeros_like(pseg)
    prev[:, 1:] = pseg[:, :-1]
    nxt[:, :-1] = pseg[:, 1:]
    maps = []
    for core in range(NCORES):
        ch = slice(core * 64, (core + 1) * 64)
        cols = np.concatenate([np.arange(k * 512 + core * 64, k * 512 + (core + 1) * 64) for k in range(3)])
        U3 = np.stack([a[:, :, cols].reshape(B, L, 3, 64).transpose(1, 2, 0, 3).reshape(L, 3 * B * 64) for a in (prev, pseg, nxt)], 0)
        cw = np.stack([np.broadcast_to(v_[cols].reshape(3, 1, 64), (3, B, 64)).reshape(-1) for v_ in (conv_w[0], conv_w[1], conv_w[2], conv_b)], 0)
        fw = np.zeros((128, 384), np.float32)
        fw[:33, 0:64] = f_w1
        fw[:64, 64:128] = f_w2
        fw[:64, 128:384] = f_w3.reshape(64, 2, 2, 512)[:, :, :, ch].reshape(64, 256)
        fcv = np.zeros((128, 4), np.float32)
        fcv[:64] = np.stack([f_freq[0], f_b1, f_freq[1], f_b2], 1)
        maps.append({"zT": C["zT"], "win": np.ascontiguousarray(C["win"][:, ch]), "tabC": C["tabC"], "tabS": C["tabS"],
                     "tabCi": C["tabCi"], "tabSi": C["tabSi"], "cfn": C["cfn"], "U3": np.ascontiguousarray(U3),
                     "cw": np.ascontiguousarray(cw), "fw": fw, "fc": fcv,
                     "hb": np.ascontiguousarray(np.broadcast_to(hy_b[:, None, ch], (2, B, 64)).reshape(2, B * 64))})
    res = launch(("hyena", L), lambda: build_hyena(L), maps)
    hy = np.zeros((B, L, 512), np.float32)
    for core in range(NCORES):
        hy[:, :, core * 64:(core + 1) * 64] = res[core]["hy"].reshape(L, B, 64).transpose(1, 0, 2)
    return hy


def kernel(x, c, ctx, c_ctx, ada_w, ada_b, norm_g, final_g, ev_w_in, ev_w_out,
           rw_mu, rw_w0, rw_w2, rw_a0, rw_a2, rw_g2, rw_kk, rw_ka, rw_rk, rw_gn,
           hy_conv_w, hy_conv_b, hy_f_w1, hy_f_b1, hy_f_w2, hy_f_b2, hy_f_w3, hy_freq, hy_bias,
           da_w_qkv, da_w_out, da_lambda, da_subln, moe_router, moe_w1, moe_w3, moe_w2):
    f = lambda a: np.ascontiguousarray(np.asarray(a, dtype=np.float32))
    (x, c, ctx, c_ctx, ada_w, ada_b, norm_g, final_g, ev_w_in, ev_w_out, rw_mu, rw_w0, rw_w2, rw_a0, rw_a2, rw_g2,
     rw_kk, rw_ka, rw_rk, rw_gn, hy_conv_w, hy_conv_b, hy_f_w1, hy_f_b1, hy_f_w2, hy_f_b2, hy_f_w3, hy_freq, hy_bias,
     da_w_qkv, da_w_out, da_lambda, da_subln, moe_router, moe_w1, moe_w3, moe_w2) = [f(a) for a in (
        x, c, ctx, c_ctx, ada_w, ada_b, norm_g, final_g, ev_w_in, ev_w_out, rw_mu, rw_w0, rw_w2, rw_a0, rw_a2, rw_g2,
        rw_kk, rw_ka, rw_rk, rw_gn, hy_conv_w, hy_conv_b, hy_f_w1, hy_f_b1, hy_f_w2, hy_f_b2, hy_f_w3, hy_freq, hy_bias,
        da_w_qkv, da_w_out, da_lambda, da_subln, moe_router, moe_w1, moe_w3, moe_w2)]
    mods = run_mod(c, c_ctx, ada_w, ada_b)
    xcur = np.ascontiguousarray(np.concatenate([x.reshape(-1, D), ctx.reshape(-1, D)], 0))
    prev = None
    NL = B * SEQ
    for i in range(4):
        j = i // 2
        if i % 2 == 0:
            xcur, p = run_tok_a(xcur, mods[i], norm_g[i, 0], ev_w_in[j], prev=prev)
            q = run_rwprep(p, rw_mu[j], rw_w0[j], rw_w2[j], rw_a0[j], rw_a2[j], rw_g2[j], rw_kk[j], rw_ka[j], rw_rk[j])
            yf, yb = run_rwscan(q)
            rw = run_rwout(yf, yb, q, rw_gn[j])
            hargs = (hy_conv_w[j], hy_conv_b[j], hy_f_w1[j], hy_f_b1[j], hy_f_w2[j], hy_f_b2[j], hy_f_w3[j], hy_freq[j], hy_bias[j])
            hyl = run_hyena(np.ascontiguousarray(p[:NL, RWC:].reshape(B, SEQ, 1536)), SEQ, *hargs)
            hyc = run_hyena(np.ascontiguousarray(p[NL:, RWC:].reshape(B, CTXL, 1536)), CTXL, *hargs)
            mix = np.ascontiguousarray(np.concatenate([rw, np.concatenate([hyl.reshape(-1, 512), hyc.reshape(-1, 512)], 0)], 1))
            w_out = ev_w_out[j]
        else:
            lam_init = 0.8 - 0.6 * math.exp(-0.3 * i)
            xcur, p = run_tok_a(xcur, mods[i], norm_g[i, 0], da_w_qkv[j], prev=prev, rope=True, pbf=True)
            mix = run_attn(p, da_lambda[j], da_subln[j], lam_init)
            if i < 3:
                mix[NL:] = run_attn_ctx(p, da_lambda[j], da_subln[j], lam_init)
            w_out = da_w_out[j]
        x1, h2, aff = run_tok_c(mix, xcur, mods[i], norm_g[i, 1], w_out, moe_router[i])
        G = run_topk(aff)
        y = run_moe(h2, G, moe_w1[i], moe_w3[i], moe_w2[i])
        xcur, prev = x1, (y, mods[i])
    zero_mod = np.zeros((5, 6 * D), np.float32)
    _, out = run_tok_a(xcur, zero_mod, final_g, None, prev=prev, final=True)
    return np.ascontiguousarray(out[:NL].reshape(B, SEQ, D).astype(np.float32))
```
